# Optimizing a Trainium2 kernel written in Bass

```python
import jax, jax.numpy as jnp
from jax import lax
import numpy as np

D_MODEL = 2048
BATCH = 2
SEQ = 4096
DEPTH = 4

HEAD_DIM = 128
CONV_CH = 512
CONV_WIDTH = 3
DSA_HEADS = 6
IDX_HEADS = 16
IDX_DIM = 64
TOPK_MAX = 256
DIL_PATTERNS = ((128, 1), (512, 4), (2048, 16))
DIL_GROUPS = 3
DIL_HEADS_PER_GROUP = 2
DIL_HEADS = DIL_GROUPS * DIL_HEADS_PER_GROUP
D_MIX = CONV_CH + DSA_HEADS * HEAD_DIM + DIL_HEADS * HEAD_DIM
SPLITS = (
    CONV_CH, CONV_CH, CONV_CH,
    DSA_HEADS * HEAD_DIM, HEAD_DIM, HEAD_DIM,
    IDX_HEADS * IDX_DIM, IDX_DIM, IDX_HEADS,
    DIL_HEADS * HEAD_DIM, DIL_HEADS * HEAD_DIM, DIL_HEADS * HEAD_DIM,
)
D_IN = sum(SPLITS)
D_FF = 5632
ROPE_THETA = 10000.0
RMS_EPS = 1e-6
Q_BLOCK = 128

kernel_name = "hybrid_conv_dsa_dilated_macaron"


def rmsnorm(x, g):
    x32 = x.astype(jnp.float32)
    y = x32 * lax.rsqrt(jnp.mean(x32 * x32, axis=-1, keepdims=True) + RMS_EPS)
    return (y * g.astype(jnp.float32)).astype(x.dtype)


def swiglu(x, w_gate, w_up, w_down):
    return (jax.nn.silu(x @ w_gate) * (x @ w_up)) @ w_down


def rope_tables(positions, dim):
    inv = 1.0 / (ROPE_THETA ** (jnp.arange(0, dim, 2, dtype=jnp.float32) / dim))
    ang = positions.astype(jnp.float32)[..., None] * inv
    return jnp.cos(ang), jnp.sin(ang)


def apply_rope(x, cos, sin):
    extra = x.ndim - 3
    shp = cos.shape[:2] + (1,) * extra + cos.shape[-1:]
    c, s = cos.reshape(shp), sin.reshape(shp)
    x1, x2 = jnp.split(x.astype(jnp.float32), 2, axis=-1)
    return jnp.concatenate([x1 * c - x2 * s, x2 * c + x1 * s], axis=-1).astype(x.dtype)


def to_blocks(a):
    b, s = a.shape[:2]
    return jnp.moveaxis(a.reshape((b, s // Q_BLOCK, Q_BLOCK) + a.shape[2:]), 1, 0)


def from_blocks(a):
    nb, b, qb, f = a.shape
    return jnp.moveaxis(a, 0, 1).reshape(b, nb * qb, f)


def short_conv_mixer(h, gate_b, gate_c, conv_w):
    u = gate_c * h
    s = u.shape[1]
    u_pad = jnp.pad(u, ((0, 0), (CONV_WIDTH - 1, 0), (0, 0)))
    y = conv_w[0] * u_pad[:, 0:s]
    for j in range(1, CONV_WIDTH):
        y = y + conv_w[j] * u_pad[:, j:j + s]
    return gate_b * y


def dsa_attention(q, k, v, q_idx, k_idx, w_idx):
    b, s = q.shape[:2]
    topk = min(TOPK_MAX, s // 4)
    scale = HEAD_DIM ** -0.5
    key_pos = jnp.arange(s)
    gather = jax.vmap(lambda kk, ii: kk[ii])

    def block(args):
        q_blk, qi_blk, w_blk, start = args
        t = start + jnp.arange(Q_BLOCK)
        causal = key_pos[None, :] <= t[:, None]
        logits = jnp.einsum('bqhd,bsd->bqhs', qi_blk, k_idx)
        score = jnp.einsum('bqh,bqhs->bqs', w_blk, jax.nn.relu(logits)).astype(jnp.float32)
        score = jnp.where(causal[None], score, -jnp.inf)
        _, sel = lax.top_k(score, topk)
        valid = sel <= t[None, :, None]
        k_sel = gather(k, sel)
        v_sel = gather(v, sel)
        att = jnp.einsum('bqhd,bqkd->bqhk', q_blk, k_sel).astype(jnp.float32) * scale
        att = jnp.where(valid[:, :, None, :], att, -jnp.inf)
        p = jax.nn.softmax(att, axis=-1)
        o = jnp.einsum('bqhk,bqkd->bqhd', p.astype(v.dtype), v_sel)
        return o.reshape(b, Q_BLOCK, DSA_HEADS * HEAD_DIM)

    starts = jnp.arange(s // Q_BLOCK) * Q_BLOCK
    out = lax.map(block, (to_blocks(q), to_blocks(q_idx), to_blocks(w_idx), starts))
    return from_blocks(out)


def dilated_attention(q, k, v):
    b, s = q.shape[:2]
    scale = HEAD_DIM ** -0.5
    k_groups = [k[:, :, g] for g in range(DIL_GROUPS)]
    v_groups = [v[:, :, g] for g in range(DIL_GROUPS)]

    def block(args):
        q_blk, start = args
        t = start + jnp.arange(Q_BLOCK)
        outs, lses = [], []
        for g, (win, dil) in enumerate(DIL_PATTERNS):
            taps = jnp.arange(win // dil + 1) * dil
            idx = t[:, None] - taps[None, :]
            valid = idx >= 0
            idx = jnp.maximum(idx, 0)
            kg = k_groups[g][:, idx]
            vg = v_groups[g][:, idx]
            sc = jnp.einsum('bqhd,bqjhd->bqhj', q_blk[:, :, g], kg).astype(jnp.float32) * scale
            sc = jnp.where(valid[None, :, None, :], sc, -jnp.inf)
            lse = jax.nn.logsumexp(sc, axis=-1)
            p = jnp.exp(sc - lse[..., None])
            outs.append(jnp.einsum('bqhj,bqjhd->bqhd', p.astype(vg.dtype), vg))
            lses.append(lse)
        o = jnp.stack(outs, axis=2)
        alpha = jax.nn.softmax(jnp.stack(lses, axis=2), axis=2)
        o = o * alpha[..., None].astype(o.dtype)
        return o.reshape(b, Q_BLOCK, DIL_HEADS * HEAD_DIM)

    starts = jnp.arange(s // Q_BLOCK) * Q_BLOCK
    out = lax.map(block, (to_blocks(q), starts))
    return from_blocks(out)


def hybrid_mixer(xn, w_in, conv_w, w_out, cos_h, sin_h, cos_i, sin_i):
    b, s, _ = xn.shape
    proj = xn @ w_in
    offsets = np.cumsum(np.array(SPLITS))[:-1].tolist()
    (h, g_b, g_c, q_b, k_b, v_b, q_i, k_i, w_i, q_c, k_c, v_c) = jnp.split(proj, offsets, axis=-1)

    y_a = short_conv_mixer(h, g_b, g_c, conv_w)

    q_b = apply_rope(q_b.reshape(b, s, DSA_HEADS, HEAD_DIM), cos_h, sin_h)
    k_b = apply_rope(k_b, cos_h, sin_h)
    q_i = apply_rope(q_i.reshape(b, s, IDX_HEADS, IDX_DIM), cos_i, sin_i) * (IDX_DIM ** -0.5)
    k_i = apply_rope(k_i, cos_i, sin_i)
    w_i = w_i * (IDX_HEADS ** -0.5)
    y_b = dsa_attention(q_b, k_b, v_b, q_i, k_i, w_i)

    gshape = (b, s, DIL_GROUPS, DIL_HEADS_PER_GROUP, HEAD_DIM)
    q_c = apply_rope(q_c.reshape(gshape), cos_h, sin_h)
    k_c = apply_rope(k_c.reshape(gshape), cos_h, sin_h)
    y_c = dilated_attention(q_c, k_c, v_c.reshape(gshape))

    return jnp.concatenate([y_a, y_b, y_c], axis=-1) @ w_out


def setup_inputs(seed: int = 0) -> dict:
    key = jax.random.key(seed)
    ks = jax.random.split(key, 16)
    nrm = lambda k, shp, fan: jax.random.normal(k, shp, jnp.float32) * (fan ** -0.5)
    gain = lambda k, shp: 1.0 + 0.02 * jax.random.normal(k, shp, jnp.float32)
    x = jax.random.normal(ks[0], (BATCH, SEQ, D_MODEL), jnp.float32)
    offs = jax.random.randint(ks[1], (BATCH, 1), 0, 1024, dtype=jnp.int32)
    positions = offs + jnp.arange(SEQ, dtype=jnp.int32)[None, :]
    return {
        "x": x,
        "positions": positions,
        "norm_ffn1": gain(ks[2], (DEPTH, D_MODEL)),
        "ffn1_gate": nrm(ks[3], (DEPTH, D_MODEL, D_FF), D_MODEL),
        "ffn1_up": nrm(ks[4], (DEPTH, D_MODEL, D_FF), D_MODEL),
        "ffn1_down": nrm(ks[5], (DEPTH, D_FF, D_MODEL), D_FF),
        "norm_mix": gain(ks[6], (DEPTH, D_MODEL)),
        "w_in": nrm(ks[7], (DEPTH, D_MODEL, D_IN), D_MODEL),
        "conv_w": nrm(ks[8], (DEPTH, CONV_WIDTH, CONV_CH), CONV_WIDTH),
        "w_out": nrm(ks[9], (DEPTH, D_MIX, D_MODEL), D_MIX),
        "norm_ffn2": gain(ks[10], (DEPTH, D_MODEL)),
        "ffn2_gate": nrm(ks[11], (DEPTH, D_MODEL, D_FF), D_MODEL),
        "ffn2_up": nrm(ks[12], (DEPTH, D_MODEL, D_FF), D_MODEL),
        "ffn2_down": nrm(ks[13], (DEPTH, D_FF, D_MODEL), D_FF),
        "norm_final": gain(ks[14], (D_MODEL,)),
    }


def reference(x, positions, norm_ffn1, ffn1_gate, ffn1_up, ffn1_down, norm_mix, w_in, conv_w, w_out,
              norm_ffn2, ffn2_gate, ffn2_up, ffn2_down, norm_final):
    cos_h, sin_h = rope_tables(positions, HEAD_DIM)
    cos_i, sin_i = rope_tables(positions, IDX_DIM)
    for i in range(DEPTH):
        x = x + 0.5 * swiglu(rmsnorm(x, norm_ffn1[i]), ffn1_gate[i], ffn1_up[i], ffn1_down[i])
        x = x + hybrid_mixer(rmsnorm(x, norm_mix[i]), w_in[i], conv_w[i], w_out[i],
                             cos_h, sin_h, cos_i, sin_i)
        x = x + 0.5 * swiglu(rmsnorm(x, norm_ffn2[i]), ffn2_gate[i], ffn2_up[i], ffn2_down[i])
    return rmsnorm(x, norm_final)
```

```python
import contextlib
import numpy as np
import ml_dtypes
import concourse.bass as bass
import concourse.mybir as mybir
from concourse.bass_utils import run_bass_kernel_spmd

F32 = mybir.dt.float32
BF16 = mybir.dt.bfloat16
I32 = mybir.dt.int32
AF = mybir.ActivationFunctionType
ALU = mybir.AluOpType
AX = mybir.AxisListType
NPBF = ml_dtypes.bfloat16

PE, ACT, DVE, POOL, SP = "tensor", "scalar", "vector", "gpsimd", "sync"
COMPUTE = (PE, ACT, DVE, POOL)

D = 2048
DFF = 5632
DIN = 5968
DEPTH = 4
T = 1024
KC = 16
NSLOT = 8
NJ = 44
G = 11
SEQ = 4096
NCORE = 8
XROWS = 1864
R_KB, R_KI, R_KC, R_VB, R_VC, R_UH = 0, 128, 192, 960, 1088, 1856
NIT = 26
TOPK = 256
ATT_SCALE = 128 ** -0.5
DIL_X = (1, 4, 16)
DIL_WIN = (128, 512, 2048)
DIL_DIL = (1, 4, 16)
DIL_LIST = [(g, kp) for g in range(3) for kp in range(DIL_X[g] + 4)]
NDIL = len(DIL_LIST)
C_H, C_GB, C_GC, C_QB, C_KB, C_VB, C_QI, C_KI, C_WI, C_QC, C_KC, C_VC = (
    0, 512, 1024, 1536, 2304, 2432, 2560, 3584, 3648, 3664, 4432, 5200)
CB_ONES, CB_ID, CB_RH, CB_RI, CB_DIL = 0, 128, 256, 384, 512
NCB = 512 + NDIL * 128
CF_GAIN, CF_CONV, CF_INV, CF_SEL, CF_PW2, CF_DSA = 0, 208, 256, 258, 262, 262 + NIT
NCF = CF_DSA + 512
NEG = -3.0e38
WSHARD = False
import os
DEBUG = os.environ.get('K_DEBUG', '').split(',')


class Buf:
    __slots__ = ("name", "writer", "readers", "excl")

    def __init__(self, name="", excl=False):
        self.name = name
        self.writer = None
        self.readers = []
        self.excl = excl


class Op:
    __slots__ = ("stream", "fn", "deps", "is_dma", "sidx", "need_inc", "tok",
                 "dsem", "dval", "waits", "prewait", "barred", "dkey", "is_cc")

    def __init__(self, stream, fn, is_dma, is_cc=False):
        self.stream = stream
        self.fn = fn
        self.is_dma = is_dma
        self.is_cc = is_cc
        self.deps = []
        self.need_inc = False
        self.tok = None
        self.waits = []
        self.prewait = None
        self.barred = False
        self.dkey = None


class Prog:
    NDSEM = 12

    def __init__(self, nc):
        self.nc = nc
        self.ops = {s: [] for s in (PE, ACT, DVE, POOL, SP)}
        self.all_ops = []
        self.stack = contextlib.ExitStack()

    def sbuf(self, name, shape, dt):
        return self.stack.enter_context(self.nc.sbuf_tensor(name, list(shape), dt))[:]

    def psum(self, name, shape, dt):
        return self.stack.enter_context(self.nc.psum_tensor(name, list(shape), dt))[:]

    def op(self, stream, fn, reads=(), writes=(), dma=False, cc=False):
        o = Op(stream, fn, dma or cc, cc)
        if cc:
            if not hasattr(self, "cc_chain"):
                self.cc_chain = Buf("cc_chain")
            writes = list(writes) + [self.cc_chain]
        deps = set()
        ex = [b for b in reads if b.excl]
        if ex:
            reads = [b for b in reads if not b.excl]
            writes = list(writes) + [b for b in ex if b not in writes]
        for b in reads:
            if b.writer is not None:
                deps.add(b.writer)
        for b in writes:
            if b.writer is not None:
                deps.add(b.writer)
            for r in b.readers:
                deps.add(r)
        o.deps = list(deps)
        for b in reads:
            b.readers.append(o)
        for b in writes:
            b.writer = o
            b.readers = []
        o.sidx = len(self.ops[stream])
        self.ops[stream].append(o)
        self.all_ops.append(o)
        return o

    def dma(self, stream, out, in_, reads=(), writes=()):
        return self.op(stream, lambda e: e.dma_start(out=out, in_=in_), reads=reads, writes=writes, dma=True)

    def barrier(self):
        lasts = [self.ops[s][-1] for s in self.ops if self.ops[s]]
        dmas = [o for o in self.all_ops if o.is_dma and not o.barred and not o.is_cc]
        for o in dmas:
            o.barred = True
        for s in self.ops:
            o = Op(s, None, False)
            o.deps = list(lasts) + list(dmas)
            o.sidx = len(self.ops[s])
            self.ops[s].append(o)
            self.all_ops.append(o)

    def emit(self):
        nc = self.nc
        st = self.stack
        sems = {s: st.enter_context(nc.semaphore("s_" + s)) for s in COMPUTE}
        dsems = {q: [st.enter_context(nc.semaphore("d_%s_%d" % (q, i))) for i in range(self.NDSEM)]
                 for q in (SP, POOL)}
        dsems["cc"] = [st.enter_context(nc.semaphore("cc_%d" % i)) for i in range(self.NDSEM)]
        dcount = {q: [0] * self.NDSEM for q in dsems}
        dnext = {q: 0 for q in dsems}
        for s, lst in self.ops.items():
            seen_c = {}
            seen_d = {}
            for o in lst:
                if o.is_dma:
                    q = "cc" if o.is_cc else s
                    inc = 1 if o.is_cc else 16
                    j = dnext[q]
                    dnext[q] = (j + 1) % self.NDSEM
                    prev = dcount[q][j]
                    if prev > 0 and seen_d.get((q, j), 0) < inc * prev:
                        o.prewait = (dsems[q][j], inc * prev)
                        seen_d[(q, j)] = inc * prev
                    dcount[q][j] = prev + 1
                    o.dsem = dsems[q][j]
                    o.dval = inc * (prev + 1)
                    o.dkey = (q, j)
                for d in o.deps:
                    if d.is_dma or d.fn is None:
                        continue
                    if d.stream == s and s == PE:
                        continue
                    if seen_c.get(d.stream, -1) >= d.sidx:
                        continue
                    seen_c[d.stream] = d.sidx
                    d.need_inc = True
                    o.waits.append(("c", d))
        for s, lst in self.ops.items():
            seen_d = {}
            for o in lst:
                if o.prewait is not None:
                    seen_d[o.dkey] = max(seen_d.get(o.dkey, 0), o.prewait[1])
                for d in o.deps:
                    if not d.is_dma:
                        continue
                    if seen_d.get(d.dkey, 0) >= d.dval:
                        continue
                    seen_d[d.dkey] = d.dval
                    o.waits.append(("d", d))
        for s in COMPUTE:
            c = 0
            for o in self.ops[s]:
                if o.fn is not None and not o.is_dma and o.need_inc:
                    c += 1
                    o.tok = c
        self.stats = {s: len(l) for s, l in self.ops.items()}
        block = st.enter_context(nc.Block())

        def run(stream):
            def body(eng):
                for o in self.ops[stream]:
                    if o.prewait is not None:
                        eng.wait_ge(o.prewait[0], o.prewait[1])
                    for kind, d in o.waits:
                        if kind == "c":
                            eng.wait_ge(sems[d.stream], d.tok)
                        else:
                            eng.wait_ge(d.dsem, d.dval)
                    if o.fn is None:
                        continue
                    ins = o.fn(eng)
                    if o.is_cc:
                        ins.then_inc(o.dsem)
                    elif o.is_dma:
                        ins.then_inc(o.dsem, 16)
                    elif o.need_inc:
                        ins.then_inc(sems[stream], 1)
            return body

        block.tensor(run(PE))
        block.vector(run(DVE))
        block.scalar(run(ACT))
        block.gpsimd(run(POOL))
        block.sync(run(SP))

    def close(self):
        self.stack.close()


class Arena:
    def __init__(self, P, nbytes):
        self.t = P.sbuf("arena", [128, nbytes // 2], BF16)
        self.nbytes = nbytes
        self.off = 0

    def reset(self, off=0):
        self.off = off

    def take(self, free_shape, dt):
        sz = 2 if dt == BF16 else 4
        n = int(np.prod(free_shape))
        nb = n * sz
        nb_al = (nb + 63) // 64 * 64
        assert self.off + nb_al <= self.nbytes, ("arena overflow", self.off, nb_al, self.nbytes)
        ap = self.t[:, self.off // 2:(self.off + nb) // 2]
        if dt != BF16:
            ap = ap.bitcast(dt)
        self.off += nb_al
        if len(free_shape) == 2:
            ap = ap.rearrange("p (a b) -> p a b", b=free_shape[1])
        elif len(free_shape) == 3:
            ap = ap.rearrange("p (a b c) -> p a b c", b=free_shape[1], c=free_shape[2])
        elif len(free_shape) == 4:
            ap = ap.rearrange("p (a b c d) -> p a b c d", b=free_shape[1], c=free_shape[2], d=free_shape[3])
        return ap


class Builder:
    def __init__(self, mode, layer=0):
        self.mode = mode
        self.layer = layer
        nc = bass.Bass("TRN2", target_bir_lowering=False)
        self.nc = nc
        self.P = P = Prog(nc)
        fused = mode == "F"
        nl = DEPTH if fused else 1

        def ext_in(name, shape, dt):
            return nc.dram_tensor(name, list(shape), dt, kind="ExternalInput").ap()

        def ext_out(name, shape, dt):
            return nc.dram_tensor(name, list(shape), dt, kind="ExternalOutput").ap()

        def scratch(name, shape, dt, produced_by):
            if fused:
                return nc.dram_tensor(name, list(shape), dt).ap()
            if produced_by == mode:
                return ext_out(name, shape, dt)
            return ext_in(name, shape, dt)

        self.wbuf = {}

        def weight(name, rows, cols):
            if not WSHARD:
                ap = ext_in(name, [nl, rows, cols], F32)
                for l in range(nl):
                    self.wbuf[(name, l)] = Buf(name)
                return ap
            shard = ext_in(name, [nl, rows // NCORE, cols], F32)
            bounce = nc.dram_tensor(name + "_bnc", [nl, rows // NCORE, cols], F32).ap()
            full = nc.dram_tensor(name + "_full", [nl, rows, cols], F32).ap()
            for l in range(nl):
                bb_ = Buf(name + "_b%d" % l)
                fb = Buf(name + "_f%d" % l)
                self.wbuf[(name, l)] = fb
                self.wgather.append((l, name, shard[l], bounce[l], full[l], bb_, fb))
            return full
        self.wgather = []
        self.cb_in = ext_in("cst_bf", [128, NCB], BF16)
        self.cf_in = ext_in("cst_f32", [128, NCF], F32)
        if mode in ("A", "F"):
            self.x_in = ext_in("x_in", [KC, 128, T], F32)
            self.pos_in = ext_in("pos", [1, T], I32)
            if "noffn" not in DEBUG:
                self.w_ffn1 = [weight("ffn1_gate", D, DFF), weight("ffn1_up", D, DFF), weight("ffn1_down", DFF, D)]
                self.n_ffn1 = ["ffn1_gate", "ffn1_up", "ffn1_down"]
            self.w_inp = weight("w_in", D, DIN)
        if mode in ("B", "F"):
            self.w_outp = weight("w_out", D, D)
            if "noffn2" not in DEBUG:
                self.w_ffn2 = [weight("ffn2_gate", D, DFF), weight("ffn2_up", D, DFF), weight("ffn2_down", DFF, D)]
                self.n_ffn2 = ["ffn2_gate", "ffn2_up", "ffn2_down"]
            self.x_out = ext_out("x_out", [KC, 128, T], F32)
        self.x_d = scratch("x_d", [KC, 128, T], F32, "A")
        self.qb_d = scratch("qb_d", [6, 128, T], BF16, "A")
        self.qi_d = scratch("qi_d", [8, 128, T], BF16, "A")
        self.qc_d = scratch("qc_d", [6, 128, T], BF16, "A")
        self.wi_d = scratch("wi_d", [NSLOT, 128, 16], F32, "A")
        self.u_d = scratch("u_d", [4, 128, T], BF16, "A")
        self.gb_d = scratch("gb_d", [4, 128, T], BF16, "A")
        if mode in ("A", "F"):
            self.xchg = scratch("xchg", [XROWS, T], BF16, "A")
        if fused:
            self.gath = nc.dram_tensor("gath", [4 * XROWS, T], BF16).ap()
        elif mode == "B":
            self.gath = ext_in("gath", [4 * XROWS, T], BF16)
        self.b_q = Buf("qside")
        self.b_xchg = Buf("xchg")
        self.b_gath = Buf("gath")

        self.cb = P.sbuf("cb", [128, NCB], BF16)
        self.cf = P.sbuf("cf", [128, NCF], F32)
        self.b_cst = Buf("cst")
        self.eps = P.sbuf("epsc", [128, 1], F32)
        self.banks = [P.psum("bank%d" % i, [128, 512], F32) for i in range(8)]
        self.bb = [Buf("bank%d" % i, excl=True) for i in range(8)]
        self.bank_rr = 0
        remaining = int(nc.sbuf_bytes_remaining)
        per_part = remaining // 128 if remaining > 400 * 1024 else remaining
        self.arena_bytes = (per_part - 1024) // 64 * 64
        self.A = Arena(P, self.arena_bytes)
        P.dma(SP, self.cb, self.cb_in, writes=[self.b_cst])
        P.dma(SP, self.cf, self.cf_in, writes=[self.b_cst])
        P.op(DVE, lambda e: e.memset(self.eps, 1e-6), writes=[self.b_cst])
        if mode == "F":
            key = lambda t: (t[0], {"ffn1_gate": 0, "ffn1_up": 1, "ffn1_down": 2, "w_in": 3, "w_out": 4,
                                    "ffn2_gate": 5, "ffn2_up": 6, "ffn2_down": 7}[t[1]])
        else:
            key = lambda t: {"ffn1_gate": 0, "ffn1_up": 1, "ffn1_down": 2, "w_in": 3, "w_out": 4,
                             "ffn2_gate": 5, "ffn2_up": 6, "ffn2_down": 7}[t[1]]
        self.wgather.sort(key=key)
        for _ in range(4):
            self.issue_gather()
        self.ones = self.cb[:, CB_ONES:CB_ONES + 128]
        self.ident = self.cb[:, CB_ID:CB_ID + 128]
        self.xb = [Buf("x%d" % c) for c in range(KC)]
        self.A.reset(0)
        self.xT = self.A.take([KC, T], F32)
        self.b_xd = [Buf("x_d%d" % c) for c in range(KC)]

    def issue_gather(self):
        if not self.wgather:
            return
        (l, name, shard, bounce, full, bb_, fb) = self.wgather.pop(0)
        P = self.P
        P.dma(SP, bounce, shard, writes=[bb_])
        P.op(POOL, lambda e: e.collective_compute("AllGather", ALU.bypass, replica_groups=[list(range(NCORE))],
                                                 ins=[bounce.opt()], outs=[full.opt()]),
             reads=[bb_], writes=[fb], cc=True)

    def nb(self):
        i = self.bank_rr
        self.bank_rr = (i + 1) % 8
        return i

    def gain(self, n):
        return self.cf[:, CF_GAIN + n * 16:CF_GAIN + (n + 1) * 16]

    def carve_x(self):
        pass

    def load_x(self, src):
        for c in range(KC):
            self.P.dma(SP, self.xT[:, c, :], src[c], reads=[self.b_xd[c]], writes=[self.xb[c]])

    def store_x(self, dst, buf=None):
        for c in range(KC):
            self.P.dma(SP, dst[c], self.xT[:, c, :], reads=[self.xb[c]], writes=[buf[c]] if buf is not None else [])

    def rmsnorm(self, gain_ap, out_fn):
        P, A = self.P, self.A
        xT = self.xT
        sq = [A.take([T], BF16) for _ in range(2)]
        sqb = [Buf("sq0"), Buf("sq1")]
        rstd = A.take([T], F32)
        rstd2 = A.take([T], F32)
        rb, rb2 = Buf("rstd"), Buf("rstd2")
        b0, b1 = self.nb(), self.nb()
        bk = (b0, b1)
        for c in range(KC):
            s = c % 2
            P.op(ACT, lambda e, c=c, s=s: e.activation(out=sq[s], in_=xT[:, c, :], func=AF.Square),
                 reads=[self.xb[c]], writes=[sqb[s]])
            for tt in range(2):
                P.op(PE, lambda e, c=c, s=s, tt=tt: e.matmul(self.banks[bk[tt]], self.ones, sq[s][:, tt * 512:(tt + 1) * 512],
                                                            start=(c == 0), stop=(c == KC - 1)),
                     reads=[sqb[s], self.b_cst], writes=[self.bb[bk[tt]]])
        for tt in range(2):
            P.op(ACT, lambda e, tt=tt: e.activation(out=rstd[:, tt * 512:(tt + 1) * 512], in_=self.banks[bk[tt]],
                                                   func=AF.Sqrt, scale=1.0 / D, bias=self.eps[:, 0:1]),
                 reads=[self.bb[bk[tt]], self.b_cst], writes=[rb])
        P.op(DVE, lambda e: e.reciprocal(out=rstd2, in_=rstd), reads=[rb], writes=[rb2])
        for c in range(KC):
            out_fn(c, rstd2, rb2, gain_ap)

    def ffn(self, wts, l, gain_n, names=None):
        P, A = self.P, self.A
        xT = self.xT
        A.reset(KC * T * 4)
        xn = A.take([KC, T], BF16)
        xnb = [Buf("xn%d" % c) for c in range(KC)]
        mark = A.off

        def norm_out(c, rstd, rb, g):
            P.op(DVE, lambda e: e.scalar_tensor_tensor(out=xn[:, c, :], in0=xT[:, c, :], scalar=g[:, c:c + 1], in1=rstd,
                                                      op0=ALU.mult, op1=ALU.mult),
                 reads=[self.xb[c], rb, self.b_cst], writes=[xnb[c]])
        self.rmsnorm(self.gain(gain_n), norm_out)
        P.barrier()
        A.reset(mark)
        hT = A.take([G, T], BF16)
        hb = [Buf("h%d" % j) for j in range(G)]
        NGU = 3
        gu = [(A.take([KC, 128], BF16), A.take([KC, 128], BF16)) for _ in range(NGU)]
        gub = [(Buf("wg%d" % i), Buf("wu%d" % i)) for i in range(NGU)]
        ND, QW = 2, 512
        dwt = [A.take([G, QW], BF16) for _ in range(ND)]
        dwb = [Buf("wd%d" % i) for i in range(ND)]
        stg = [A.take([512], BF16) for _ in range(2)]
        stb = [Buf("st0"), Buf("st1")]
        wgv = wts[0][l].rearrange("(kc p) n -> p kc n", p=128)
        wuv = wts[1][l].rearrange("(kc p) n -> p kc n", p=128)
        wdv = wts[2][l].rearrange("(j p) n -> p j n", p=128)
        gucnt = dcnt = stc = 0
        banks, bb = self.banks, self.bb
        for g in range(NJ // G):
            for jj in range(G):
                j = g * G + jj
                s = gucnt % NGU
                gucnt += 1
                if jj == 5:
                    self.issue_gather()
                P.dma(POOL, gu[s][0], wgv[:, :, j * 128:(j + 1) * 128], reads=[self.wbuf[(names[0], l)]], writes=[gub[s][0]])
                P.dma(POOL, gu[s][1], wuv[:, :, j * 128:(j + 1) * 128], reads=[self.wbuf[(names[1], l)]], writes=[gub[s][1]])
                bg = [self.nb(), self.nb()]
                bu = [self.nb(), self.nb()]
                for which, bk in ((0, bg), (1, bu)):
                    for kc in range(KC):
                        for tt in range(2):
                            P.op(PE, lambda e, s=s, which=which, kc=kc, tt=tt, bk=bk: e.matmul(
                                banks[bk[tt]], gu[s][which][:, kc, :], xn[:, kc, tt * 512:(tt + 1) * 512],
                                start=(kc == 0), stop=(kc == KC - 1)),
                                reads=[gub[s][which], xnb[kc]], writes=[bb[bk[tt]]])
                for tt in range(2):
                    k = stc % 2
                    stc += 1
                    P.op(ACT, lambda e, k=k, tt=tt, bg=bg: e.activation(out=stg[k], in_=banks[bg[tt]], func=AF.Silu),
                         reads=[bb[bg[tt]]], writes=[stb[k]])
                    P.op(DVE, lambda e, k=k, tt=tt, bu=bu, jj=jj: e.tensor_tensor(
                        out=hT[:, jj, tt * 512:(tt + 1) * 512], in0=stg[k], in1=banks[bu[tt]], op=ALU.mult),
                        reads=[stb[k], bb[bu[tt]]], writes=[hb[jj]])
            for q in range(D // QW):
                s = dcnt % ND
                dcnt += 1
                P.dma(POOL, dwt[s], wdv[:, g * G:(g + 1) * G, q * QW:(q + 1) * QW], reads=[self.wbuf[(names[2], l)]], writes=[dwb[s]])
                for dd in range(QW // 128):
                    dc = q * (QW // 128) + dd
                    bk = [self.nb(), self.nb()]
                    for jj in range(G):
                        for tt in range(2):
                            P.op(PE, lambda e, s=s, jj=jj, tt=tt, dd=dd, bk=bk: e.matmul(
                                banks[bk[tt]], dwt[s][:, jj, dd * 128:(dd + 1) * 128], hT[:, jj, tt * 512:(tt + 1) * 512],
                                start=(jj == 0), stop=(jj == G - 1)),
                                reads=[dwb[s], hb[jj]], writes=[bb[bk[tt]]])
                    for tt in range(2):
                        P.op(DVE, lambda e, dc=dc, tt=tt, bk=bk: e.scalar_tensor_tensor(
                            out=xT[:, dc, tt * 512:(tt + 1) * 512], in0=banks[bk[tt]], scalar=0.5,
                            in1=xT[:, dc, tt * 512:(tt + 1) * 512], op0=ALU.mult, op1=ALU.add),
                            reads=[bb[bk[tt]], self.xb[dc]], writes=[self.xb[dc]])

    def inproj(self, l):
        P, A = self.P, self.A
        xT, banks, bb = self.xT, self.banks, self.bb
        cf = self.cf
        A.reset(KC * T * 4)
        xn = A.take([KC, T], BF16)
        xnb = [Buf("xn%d" % c) for c in range(KC)]
        tabs = [A.take([T], F32) for _ in range(4)]
        tabb = Buf("tabs")
        mark = A.off
        posi = A.take([T], I32)
        posf = A.take([T], F32)
        ang = A.take([T], F32)
        kf = A.take([T], F32)
        ki = A.take([T], I32)
        y1 = A.take([T], F32)
        y2 = A.take([T], F32)
        tb = [Buf("tt%d" % i) for i in range(7)]
        P.dma(SP, posi, self.pos_in.partition_broadcast(128), writes=[tb[0]])
        P.op(DVE, lambda e: e.tensor_copy(out=posf, in_=posi), reads=[tb[0]], writes=[tb[1]])
        C1 = 6.28125
        C2 = float(2 * np.pi - 6.28125)
        for ti, (invcol, shift) in enumerate(((0, np.pi / 2), (0, 0.0), (1, np.pi / 2), (1, 0.0))):
            P.op(DVE, lambda e, invcol=invcol, shift=shift: e.tensor_scalar(
                out=ang, in0=posf, scalar1=cf[:, CF_INV + invcol:CF_INV + invcol + 1], scalar2=float(shift),
                op0=ALU.mult, op1=ALU.add), reads=[tb[1], self.b_cst], writes=[tb[2]])
            P.op(DVE, lambda e: e.tensor_scalar(out=kf, in0=ang, scalar1=float(1 / (2 * np.pi)), scalar2=None, op0=ALU.mult),
                 reads=[tb[2]], writes=[tb[3]])
            P.op(DVE, lambda e: e.tensor_copy(out=ki, in_=kf), reads=[tb[3]], writes=[tb[4]])
            P.op(DVE, lambda e: e.tensor_copy(out=kf, in_=ki), reads=[tb[4]], writes=[tb[3]])
            P.op(DVE, lambda e: e.scalar_tensor_tensor(out=y1, in0=kf, scalar=-C1, in1=ang, op0=ALU.mult, op1=ALU.add),
                 reads=[tb[3], tb[2]], writes=[tb[5]])
            P.op(DVE, lambda e: e.scalar_tensor_tensor(out=y2, in0=kf, scalar=-C2, in1=y1, op0=ALU.mult, op1=ALU.add),
                 reads=[tb[3], tb[5]], writes=[tb[6]])
            P.op(ACT, lambda e, ti=ti: e.activation(out=tabs[ti], in_=y2, func=AF.Sin, scale=1.0 - 1e-6),
                 reads=[tb[6]], writes=[tabb])
        P.barrier()
        A.reset(mark)
        if "tables_only" in DEBUG:
            return

        def norm_out(c, rstd, rb, g):
            P.op(DVE, lambda e: e.scalar_tensor_tensor(out=xn[:, c, :], in0=xT[:, c, :], scalar=g[:, c:c + 1], in1=rstd,
                                                      op0=ALU.mult, op1=ALU.mult),
                 reads=[self.xb[c], rb, self.b_cst], writes=[xnb[c]])
        self.rmsnorm(self.gain(l * 3 + 1 if self.mode == "F" else 1), norm_out)
        NW = 3
        wch = [A.take([KC, 128], BF16) for _ in range(NW)]
        wchb = [Buf("wch%d" % i) for i in range(NW)]
        ost = [A.take([T], BF16) for _ in range(3)]
        ostb = [Buf("ost%d" % i) for i in range(3)]
        hs = [A.take([T], F32) for _ in range(2)]
        hsb = [Buf("hs0"), Buf("hs1")]
        q16 = [A.take([512], BF16) for _ in range(2)]
        q16b = [Buf("q16_0"), Buf("q16_1")]
        t1 = [A.take([512], F32) for _ in range(2)]
        t1b = [Buf("t1_0"), Buf("t1_1")]
        t2 = [A.take([512], F32) for _ in range(2)]
        t2b = [Buf("t2_0"), Buf("t2_1")]
        tst = [A.take([NSLOT, 128], BF16) for _ in range(2)]
        tstb = [Buf("tst0"), Buf("tst1")]
        wst = A.take([NSLOT, 16], F32)
        wstb = Buf("wst")
        wl = self.w_inp[l if self.mode == "F" else 0]
        wv = wl.rearrange("(kc p) n -> p kc n", p=128)
        b_win = self.wbuf[("w_in", l if self.mode == "F" else 0)]
        xchg = self.xchg
        cnt = {"w": 0, "o": 0, "r": 0, "h": 0, "t": 0}
        R_h = self.cb[:, CB_RH:CB_RH + 128]
        R_i = self.cb[:, CB_RI:CB_RI + 128]

        def fm_chunk(col, M, kind, dst, dst_buf, tab=None, hslot=None, extra=None):
            s = cnt["w"] % NW
            cnt["w"] += 1
            P.dma(POOL, wch[s][:, :, 0:M], wv[:, :, col:col + M], reads=[b_win], writes=[wchb[s]])
            bk = [self.nb(), self.nb()]
            for kc in range(KC):
                for tt in range(2):
                    P.op(PE, lambda e, s=s, kc=kc, tt=tt, bk=bk: e.matmul(
                        banks[bk[tt]][0:M, :], wch[s][:, kc, 0:M], xn[:, kc, tt * 512:(tt + 1) * 512],
                        start=(kc == 0), stop=(kc == KC - 1)),
                        reads=[wchb[s], xnb[kc]], writes=[bb[bk[tt]]])
            if kind == "h":
                for tt in range(2):
                    P.op(ACT, lambda e, tt=tt, bk=bk: e.activation(out=hs[hslot][:, tt * 512:(tt + 1) * 512],
                                                                 in_=banks[bk[tt]], func=AF.Copy),
                         reads=[bb[bk[tt]]], writes=[hsb[hslot]])
                return
            o = cnt["o"] % 3
            cnt["o"] += 1
            for tt in range(2):
                sl = slice(tt * 512, (tt + 1) * 512)
                if kind == "gc":
                    P.op(DVE, lambda e, tt=tt, bk=bk, sl=sl, o=o: e.tensor_tensor(
                        out=ost[o][:, sl], in0=hs[hslot][:, sl], in1=banks[bk[tt]], op=ALU.mult),
                        reads=[hsb[hslot], bb[bk[tt]]], writes=[ostb[o]])
                elif kind == "gb":
                    P.op(ACT, lambda e, tt=tt, bk=bk, sl=sl, o=o: e.activation(out=ost[o][:, sl], in_=banks[bk[tt]], func=AF.Copy),
                         reads=[bb[bk[tt]]], writes=[ostb[o]])
                else:
                    k = cnt["r"] % 2
                    cnt["r"] += 1
                    cosT, sinT, Rm = tab
                    rb_ = self.nb()
                    if "rope_nomm" in DEBUG:
                        rb_ = bk[tt]
                    else:
                        P.op(ACT, lambda e, k=k, tt=tt, bk=bk: e.activation(out=q16[k][0:M, :], in_=banks[bk[tt]][0:M, :], func=AF.Copy),
                             reads=[bb[bk[tt]]], writes=[q16b[k]])
                        P.op(PE, lambda e, k=k, rb_=rb_, Rm=Rm: e.matmul(banks[rb_][0:M, :], Rm[0:M, 0:M], q16[k][0:M, :],
                                                                       start=True, stop=True),
                             reads=[q16b[k], self.b_cst], writes=[bb[rb_]])
                    if "rope_nomul" in DEBUG:
                        P.op(DVE, lambda e, k=k, tt=tt, bk=bk: e.tensor_copy(out=t1[k][0:M, :], in_=banks[bk[tt]][0:M, :]),
                             reads=[bb[bk[tt]]], writes=[t1b[k]])
                        P.op(DVE, lambda e, k=k, rb_=rb_: e.tensor_copy(out=t2[k][0:M, :], in_=banks[rb_][0:M, :]),
                             reads=[bb[rb_]], writes=[t2b[k]])
                    else:
                        P.op(DVE, lambda e, k=k, tt=tt, bk=bk, sl=sl, cosT=cosT: e.tensor_tensor(
                            out=t1[k][0:M, :], in0=banks[bk[tt]][0:M, :], in1=cosT[0:M, sl], op=ALU.mult),
                            reads=[bb[bk[tt]], tabb], writes=[t1b[k]])
                        P.op(DVE, lambda e, k=k, rb_=rb_, sl=sl, sinT=sinT: e.tensor_tensor(
                            out=t2[k][0:M, :], in0=banks[rb_][0:M, :], in1=sinT[0:M, sl], op=ALU.mult),
                            reads=[bb[rb_], tabb], writes=[t2b[k]])
                    P.op(DVE if "ropeadd_dve" in DEBUG else POOL, lambda e, k=k, sl=sl, o=o: e.tensor_tensor(
                        out=ost[o][0:M, sl], in0=t1[k][0:M, :], in1=t2[k][0:M, :], op=ALU.add),
                        reads=[t1b[k], t2b[k]], writes=[ostb[o]])
            P.dma(SP, dst, ost[o][0:M, :], reads=[ostb[o]], writes=[dst_buf])
            if extra is not None:
                extra(o)

        rope_h = (tabs[0], tabs[1], R_h)
        rope_i = (tabs[2], tabs[3], R_i)
        if "norm_only" in DEBUG:
            return
        uh_flat = xchg[R_UH:R_UH + 8, :].rearrange("r (a b) -> (r a) b", b=16).rearrange("(c p) (i t) -> c p i t", p=128, t=2)
        for c in range(4):
            if "noconv" in DEBUG:
                break
            hslot = c % 2
            fm_chunk(C_H + c * 128, 128, "h", None, None, hslot=hslot)

            def uh_extra(o, c=c):
                P.dma(SP, uh_flat[c], ost[o].rearrange("p (i s) -> p i s", s=128)[:, :, 126:128],
                      reads=[ostb[o]], writes=[self.b_xchg])
            fm_chunk(C_GC + c * 128, 128, "gc", self.u_d[c], self.b_q, hslot=hslot, extra=uh_extra)
            fm_chunk(C_GB + c * 128, 128, "gb", self.gb_d[c], self.b_q)
        if "conv_only" in DEBUG:
            return
        for h in range(6):
            fm_chunk(C_QB + h * 128, 128, "rope", self.qb_d[h], self.b_q, tab=rope_h)
        if "qb_only" in DEBUG:
            return
        fm_chunk(C_KB, 128, "rope", xchg[R_KB:R_KB + 128, :], self.b_xchg, tab=rope_h)
        for c in range(8):
            fm_chunk(C_QI + c * 128, 128, "rope", self.qi_d[c], self.b_q, tab=rope_i)
        fm_chunk(C_KI, 64, "rope", xchg[R_KI:R_KI + 64, :], self.b_xchg, tab=rope_i)
        for h in range(6):
            fm_chunk(C_QC + h * 128, 128, "rope", self.qc_d[h], self.b_q, tab=rope_h)
        for h in range(6):
            fm_chunk(C_KC + h * 128, 128, "rope", xchg[R_KC + h * 128:R_KC + (h + 1) * 128, :], self.b_xchg, tab=rope_h)
        if "fm_only" in DEBUG:
            return
        vb_dst = xchg[R_VB:R_VB + 128, :].rearrange("r (t d) -> (r t) d", d=128).rearrange("(i s) d -> s i d", s=128)
        vc_all = xchg[R_VC:R_VC + 768, :].rearrange("r c -> (r c)").rearrange("(t d) -> t d", d=768)
        pieces = [(C_VB, 128, "v", vb_dst)]
        for k in range(6):
            pieces.append((C_VC + k * 128, 128, "v", vc_all[:, k * 128:(k + 1) * 128].rearrange("(i s) d -> s i d", s=128)))
        pieces.append((C_WI, 16, "w", self.wi_d.rearrange("i s h -> s i h")))
        for (col, N, kind, dst) in pieces:
            s = cnt["w"] % NW
            cnt["w"] += 1
            P.dma(POOL, wch[s][:, :, 0:N], wv[:, :, col:col + N], reads=[b_win], writes=[wchb[s]])
            if kind == "v":
                k = cnt["t"] % 2
                cnt["t"] += 1
                for half in range(2):
                    bk = self.nb()
                    for ii in range(4):
                        i = half * 4 + ii
                        for kc in range(KC):
                            P.op(PE, lambda e, s=s, kc=kc, i=i, ii=ii, bk=bk: e.matmul(
                                banks[bk][:, ii * 128:(ii + 1) * 128], xn[:, kc, i * 128:(i + 1) * 128], wch[s][:, kc, 0:128],
                                start=(kc == 0), stop=(kc == KC - 1)),
                                reads=[wchb[s], xnb[kc]], writes=[bb[bk]])
                    P.op(ACT, lambda e, k=k, half=half, bk=bk: e.activation(
                        out=tst[k][:, half * 4:(half + 1) * 4, :].rearrange("p a b -> p (a b)"), in_=banks[bk], func=AF.Copy),
                        reads=[bb[bk]], writes=[tstb[k]])
                P.dma(SP, dst, tst[k], reads=[tstb[k]], writes=[self.b_xchg])
            else:
                bk = self.nb()
                for i in range(NSLOT):
                    for kc in range(KC):
                        P.op(PE, lambda e, s=s, kc=kc, i=i, bk=bk: e.matmul(
                            banks[bk][:, i * 16:(i + 1) * 16], xn[:, kc, i * 128:(i + 1) * 128], wch[s][:, kc, 0:16],
                            start=(kc == 0), stop=(kc == KC - 1)),
                            reads=[wchb[s], xnb[kc]], writes=[bb[bk]])
                P.op(ACT, lambda e, bk=bk: e.activation(out=wst.rearrange("p a b -> p (a b)"), in_=banks[bk][:, 0:128],
                                                       func=AF.Copy, scale=1.0 / 32.0),
                     reads=[bb[bk]], writes=[wstb])
                P.dma(SP, dst, wst, reads=[wstb], writes=[self.b_q])

    def attention(self, l):
        P, A = self.P, self.A
        banks, bb, cf, cb = self.banks, self.bb, self.cf, self.cb
        gath = self.gath
        A.reset(0)
        ki_e = A.take([SEQ], BF16)
        ki_o = A.take([SEQ], BF16)
        kbT = A.take([SEQ], BF16)
        vb = A.take([32, 128], BF16)
        maskT = A.take([32, 128], BF16)
        score = A.take([SEQ], F32)
        maskq = A.take([SEQ], BF16)
        assert A.off <= KC * T * 4 + 16 * 1024
        A.reset(max(A.off, KC * T * 4))
        yT = A.take([KC, T], BF16)
        self.yT = yT
        self.yb = [Buf("y%d" % i) for i in range(NSLOT)]
        uh = A.take([4, 4, NSLOT, 2], BF16)
        b_kside = Buf("kside")
        b_uh = Buf("uh")
        rl = [A.take([512], F32) for _ in range(2)]
        rlb = [Buf("rl0"), Buf("rl1")]
        pe_ = [A.take([768], BF16) for _ in range(2)]
        peb = [Buf("pe0"), Buf("pe1")]
        pm = [A.take([768], BF16) for _ in range(2)]
        pmb = [Buf("pm0"), Buf("pm1")]
        qi_s = A.take([8, 128], BF16)
        qb_s = A.take([6, 128], BF16)
        qc_s = A.take([6, 128], BF16)
        wi_s = A.take([16], F32)
        uext = A.take([4, 130], BF16)
        gb_s = A.take([4, 128], BF16)
        cacc = A.take([128], F32)
        ctmp = A.take([128], F32)
        utmp = A.take([4, 2], BF16)
        stmp = A.take([512], F32)
        b_ctmp, b_utmp, b_stmp = Buf("ctmp"), Buf("utmp"), Buf("stmp")
        b_qi, b_qb, b_qc, b_wi, b_ue, b_gb, b_cacc = (Buf(n) for n in ("qi", "qb", "qc", "wi", "ue", "gb", "cacc"))
        NDK = 4
        kc_s = [A.take([2, 128], BF16) for _ in range(NDK)]
        vc_s = [A.take([256], BF16) for _ in range(NDK)]
        kcb = [Buf("kc%d" % i) for i in range(NDK)]
        vcb = [Buf("vc%d" % i) for i in range(NDK)]
        pe2 = [A.take([256], BF16) for _ in range(2)]
        pe2b = [Buf("pe2_0"), Buf("pe2_1")]
        pm2 = [A.take([256], BF16) for _ in range(2)]
        pm2b = [Buf("pm2_0"), Buf("pm2_1")]
        rden = A.take([768], F32)
        b_rden = Buf("rden")
        sm = A.take([8], F32)
        wk = A.take([NIT], F32)
        b_sm = Buf("sm")
        b_score, b_maskq, b_maskT = Buf("score"), Buf("maskq"), Buf("maskT")
        lo, hi, Rr, mid, cn, tt_ = (sm[:, k:k + 1] for k in range(6))
        self.att_end = A.off

        P.op(DVE, lambda e: e.memset(ki_e[64:128, :], 0.0), writes=[b_kside])
        P.op(DVE, lambda e: e.memset(ki_o[0:64, :], 0.0), writes=[b_kside])
        for rk in range(4):
            base = rk * XROWS

            def seqview(ap):
                return ap.rearrange("p (i r s) -> p i r s", r=4, s=128)[:, :, rk, :]
            src_ki = gath[base + R_KI:base + R_KI + 64, :].rearrange("d (i s) -> d i s", s=128)
            P.dma(SP, seqview(ki_e[0:64, :]), src_ki, reads=[self.b_gath], writes=[b_kside])
            P.dma(SP, seqview(ki_o[64:128, :]), src_ki, reads=[self.b_gath], writes=[b_kside])
            P.dma(SP, seqview(kbT), gath[base + R_KB:base + R_KB + 128, :].rearrange("d (i s) -> d i s", s=128),
                  reads=[self.b_gath], writes=[b_kside])
            vsrc = gath[base + R_VB:base + R_VB + 128, :].rearrange("r (t d) -> (r t) d", d=128).rearrange("(i s) d -> s i d", s=128)
            P.dma(SP, vb.rearrange("p (i r) d -> p i r d", r=4)[:, :, rk, :], vsrc, reads=[self.b_gath], writes=[b_kside])
            usrc = gath[base + R_UH:base + R_UH + 8, :].rearrange("r (a b) -> (r a) b", b=16).rearrange("(c p) (i t) -> p c i t", p=128, t=2)
            P.dma(SP, uh[:, rk], usrc, reads=[self.b_gath], writes=[b_uh])

        dmask = cb[:, CB_DIL:CB_DIL + NDIL * 128].rearrange("p (n q) -> p n q", q=128)
        dsam = cf[:, CF_DSA:CF_DSA + 512]
        li = l if self.mode == "F" else 0
        dcnt = 0
        for i in range(NSLOT):
            tsl = slice(i * 128, (i + 1) * 128)
            nblk = i + 1
            nkt = 4 * i + 4
            keys = nkt * 128
            P.dma(SP, qi_s, self.qi_d[:, :, tsl].rearrange("c p t -> p c t"), reads=[self.b_q], writes=[b_qi])
            P.dma(SP, qb_s, self.qb_d[:, :, tsl].rearrange("c p t -> p c t"), reads=[self.b_q], writes=[b_qb])
            P.dma(SP, qc_s, self.qc_d[:, :, tsl].rearrange("c p t -> p c t"), reads=[self.b_q], writes=[b_qc])
            P.dma(SP, wi_s, self.wi_d[i], reads=[self.b_q], writes=[b_wi])
            P.dma(SP, uext[:, :, 2:130], self.u_d[:, :, tsl].rearrange("c p t -> p c t"), reads=[self.b_q], writes=[b_ue])
            P.dma(SP, gb_s, self.gb_d[:, :, tsl].rearrange("c p t -> p c t"), reads=[self.b_q], writes=[b_gb])
            first = True
            for j in range(4):
                if j == 0:
                    if i == 0:
                        continue
                    cand = uh[:, 3, :, i - 1, :]
                else:
                    cand = uh[:, j - 1, :, i, :]
                selj = cf[:, CF_SEL + j:CF_SEL + j + 1]
                if first:
                    P.op(POOL, lambda e, cand=cand, selj=selj: e.tensor_scalar(out=uext[:, :, 0:2], in0=cand, scalar1=selj,
                                                                              scalar2=None, op0=ALU.mult),
                         reads=[b_uh, self.b_cst], writes=[b_ue])
                    first = False
                else:
                    P.op(POOL, lambda e, cand=cand, selj=selj: e.tensor_scalar(out=utmp, in0=cand, scalar1=selj,
                                                                              scalar2=None, op0=ALU.mult),
                         reads=[b_uh, self.b_cst], writes=[b_utmp])
                    P.op(POOL, lambda e: e.tensor_tensor(out=uext[:, :, 0:2], in0=uext[:, :, 0:2], in1=utmp, op=ALU.add),
                         reads=[b_utmp, b_ue], writes=[b_ue])
            for c in range(4):
                def cw(j, c=c):
                    col = CF_CONV + (li * 3 + j) * 4 + c
                    return cf[:, col:col + 1]
                P.op(POOL, lambda e, c=c, cw=cw: e.tensor_scalar(out=cacc, in0=uext[:, c, 0:128], scalar1=cw(0), scalar2=None,
                                                                op0=ALU.mult), reads=[b_ue, self.b_cst], writes=[b_cacc])
                for j in (1, 2):
                    P.op(POOL, lambda e, c=c, j=j, cw=cw: e.tensor_scalar(out=ctmp, in0=uext[:, c, j:j + 128], scalar1=cw(j),
                                                                         scalar2=None, op0=ALU.mult),
                         reads=[b_ue, self.b_cst], writes=[b_ctmp])
                    P.op(POOL, lambda e: e.tensor_tensor(out=cacc, in0=cacc, in1=ctmp, op=ALU.add),
                         reads=[b_ctmp, b_cacc], writes=[b_cacc])
                P.op(POOL, lambda e, c=c, tsl=tsl: e.tensor_tensor(out=yT[:, c, tsl], in0=cacc, in1=gb_s[:, c, :], op=ALU.mult),
                     reads=[b_cacc, b_gb], writes=[self.yb[i]])
            for b in range(nblk):
                ksl = slice(b * 512, (b + 1) * 512)
                for h in range(16):
                    k = (b * 16 + h) % 2
                    kside = ki_e if h % 2 == 0 else ki_o
                    P.op(PE, lambda e, h=h, ksl=ksl, kside=kside: e.matmul(banks[6], qi_s[:, h // 2, :], kside[:, ksl],
                                                                          start=True, stop=True),
                         reads=[b_qi, b_kside], writes=[bb[6]])
                    P.op(ACT, lambda e, k=k: e.activation(out=rl[k], in_=banks[6], func=AF.Relu),
                         reads=[bb[6]], writes=[rlb[k]])
                    eng = DVE if h % 2 == 0 else POOL
                    if h == 0:
                        P.op(eng, lambda e, k=k, ksl=ksl: e.tensor_scalar(out=score[:, ksl], in0=rl[k], scalar1=wi_s[:, 0:1],
                                                                         scalar2=None, op0=ALU.mult),
                             reads=[rlb[k], b_wi], writes=[b_score])
                    elif eng == DVE:
                        P.op(DVE, lambda e, k=k, ksl=ksl, h=h: e.scalar_tensor_tensor(
                            out=score[:, ksl], in0=rl[k], scalar=wi_s[:, h:h + 1], in1=score[:, ksl], op0=ALU.mult, op1=ALU.add),
                            reads=[rlb[k], b_wi, b_score], writes=[b_score])
                    else:
                        P.op(POOL, lambda e, k=k, h=h: e.tensor_scalar(out=stmp, in0=rl[k], scalar1=wi_s[:, h:h + 1],
                                                                      scalar2=None, op0=ALU.mult),
                             reads=[rlb[k], b_wi], writes=[b_stmp])
                        P.op(POOL, lambda e, ksl=ksl: e.tensor_tensor(out=score[:, ksl], in0=score[:, ksl], in1=stmp, op=ALU.add),
                             reads=[b_stmp, b_score], writes=[b_score])
            sc = score[:, 0:keys]
            P.op(DVE, lambda e, sc=sc: e.tensor_reduce(out=lo, in_=sc, axis=AX.X, op=ALU.min), reads=[b_score], writes=[b_sm])
            P.op(DVE, lambda e: e.tensor_scalar(out=lo, in0=lo, scalar1=-1.0, scalar2=None, op0=ALU.add), reads=[b_sm], writes=[b_sm])
            lsl = slice(keys - 512, keys)
            P.op(DVE, lambda e, lsl=lsl: e.tensor_tensor(out=score[:, lsl], in0=score[:, lsl], in1=dsam, op=ALU.add),
                 reads=[b_score, self.b_cst, b_sm], writes=[b_score])
            P.op(DVE, lambda e, sc=sc: e.tensor_reduce(out=hi, in_=sc, axis=AX.X, op=ALU.max), reads=[b_score], writes=[b_sm])
            P.op(DVE, lambda e: e.tensor_tensor(out=Rr, in0=hi, in1=lo, op=ALU.subtract), reads=[b_sm], writes=[b_sm])
            P.op(DVE, lambda e: e.tensor_scalar(out=wk, in0=cf[:, CF_PW2:CF_PW2 + NIT], scalar1=Rr, scalar2=None, op0=ALU.mult),
                 reads=[b_sm, self.b_cst], writes=[b_sm])
            mq = maskq[:, 0:keys]
            for k in range(NIT):
                P.op(DVE, lambda e, k=k: e.tensor_tensor(out=mid, in0=lo, in1=wk[:, k:k + 1], op=ALU.add), reads=[b_sm], writes=[b_sm])
                P.op(DVE, lambda e, sc=sc, mq=mq: e.tensor_scalar(out=mq, in0=sc, scalar1=mid, scalar2=0.0, op0=ALU.is_gt,
                                                                 op1=ALU.add, accum_out=cn),
                     reads=[b_score, b_sm], writes=[b_maskq, b_sm])
                P.op(DVE, lambda e, k=k: e.tensor_scalar(out=tt_, in0=cn, scalar1=TOPK - 0.5, scalar2=wk[:, k:k + 1],
                                                        op0=ALU.is_gt, op1=ALU.mult), reads=[b_sm], writes=[b_sm])
                P.op(DVE, lambda e: e.tensor_tensor(out=lo, in0=lo, in1=tt_, op=ALU.add), reads=[b_sm], writes=[b_sm])
            P.op(DVE, lambda e, sc=sc, mq=mq: e.tensor_scalar(out=mq, in0=sc, scalar1=lo, scalar2=None, op0=ALU.is_gt),
                 reads=[b_score, b_sm], writes=[b_maskq])
            pT = banks[7].bitcast(BF16).rearrange("p (a b) -> p a b", b=128)
            for b in range(nblk):
                for j in range(4):
                    kt = b * 4 + j
                    P.op(PE, lambda e, kt=kt, j=j: e.transpose(pT[:, j, :], maskq[:, kt * 128:(kt + 1) * 128], self.ident),
                         reads=[b_maskq, self.b_cst], writes=[bb[7]])
                P.op(ACT, lambda e, b=b: e.activation(out=maskT[:, b * 4:(b + 1) * 4, :].rearrange("p a b -> p (a b)"),
                                                     in_=pT[:, 0:4, :].rearrange("p a b -> p (a b)"), func=AF.Copy),
                     reads=[bb[7]], writes=[b_maskT])
            qb_f = qb_s.rearrange("p a b -> p (a b)")
            for kt in range(nkt):
                k = kt % 2
                for half in range(2):
                    P.op(PE, lambda e, kt=kt, half=half: e.matmul(banks[half][:, 0:384], kbT[:, kt * 128:(kt + 1) * 128],
                                                                 qb_f[:, half * 384:(half + 1) * 384], start=True, stop=True),
                         reads=[b_kside, b_qb], writes=[bb[half]])
                for half in range(2):
                    P.op(ACT, lambda e, k=k, half=half: e.activation(out=pe_[k][:, half * 384:(half + 1) * 384],
                                                                    in_=banks[half][:, 0:384], func=AF.Exp, scale=ATT_SCALE),
                         reads=[bb[half]], writes=[peb[k]])
                P.op(DVE, lambda e, k=k, kt=kt: e.tensor_tensor(
                    out=pm[k].rearrange("p (a b) -> p a b", b=128), in0=pe_[k].rearrange("p (a b) -> p a b", b=128),
                    in1=maskT[:, kt, :].unsqueeze(1).to_broadcast([128, 6, 128]), op=ALU.mult),
                    reads=[peb[k], b_maskT], writes=[pmb[k]])
                for half in range(2):
                    P.op(PE, lambda e, k=k, kt=kt, half=half: e.matmul(banks[2 + half][:, 0:384], vb[:, kt, :],
                                                                      pm[k][:, half * 384:(half + 1) * 384],
                                                                      start=(kt == 0), stop=(kt == nkt - 1)),
                         reads=[b_kside, pmb[k]], writes=[bb[2 + half]])
                    P.op(PE, lambda e, k=k, kt=kt, half=half: e.matmul(banks[4 + half][:, 0:384], self.ones,
                                                                      pm[k][:, half * 384:(half + 1) * 384],
                                                                      start=(kt == 0), stop=(kt == nkt - 1)),
                         reads=[self.b_cst, pmb[k]], writes=[bb[4 + half]])
            for half in range(2):
                P.op(DVE, lambda e, half=half: e.reciprocal(out=rden[:, half * 384:(half + 1) * 384], in_=banks[4 + half][:, 0:384]),
                     reads=[bb[4 + half]], writes=[b_rden])
                P.op(DVE, lambda e, half=half, tsl=tsl: e.tensor_tensor(
                    out=yT[:, 4 + 3 * half:7 + 3 * half, tsl], in0=banks[2 + half][:, 0:384].rearrange("p (a b) -> p a b", b=128),
                    in1=rden[:, half * 384:(half + 1) * 384].rearrange("p (a b) -> p a b", b=128), op=ALU.mult),
                    reads=[bb[2 + half], b_rden], writes=[self.yb[i]])
            steps = []
            for di, (g, kp) in enumerate(DIL_LIST):
                Tk = 4 * i - DIL_X[g] + kp
                if Tk >= 0:
                    steps.append((di, g, Tk))
            started = {2: False, 3: False}
            nsteps = len(steps)
            last_of_region = {}
            for n, (di, g, Tk) in enumerate(steps):
                for hh in range(2):
                    last_of_region[2 * g + hh] = n
            for n, (di, g, Tk) in enumerate(steps):
                rk, ik = Tk % 4, Tk // 4
                base = rk * XROWS
                s = dcnt % NDK
                k = dcnt % 2
                dcnt += 1
                ksrc = gath[base + R_KC + 2 * g * 128:base + R_KC + (2 * g + 2) * 128, ik * 128:(ik + 1) * 128].rearrange(
                    "(h d) s -> d h s", d=128)
                P.dma(SP, kc_s[s], ksrc, reads=[self.b_gath], writes=[kcb[s]])
                vsrc = gath[base + R_VC:base + R_VC + 768, :].rearrange("r c -> (r c)").rearrange("(t d) -> t d", d=768)[
                    ik * 128:(ik + 1) * 128, 2 * g * 128:(2 * g + 2) * 128]
                P.dma(SP, vc_s[s], vsrc, reads=[self.b_gath], writes=[vcb[s]])
                for hh in range(2):
                    P.op(PE, lambda e, s=s, hh=hh, g=g: e.matmul(banks[0][:, hh * 128:(hh + 1) * 128], kc_s[s][:, hh, :],
                                                                qc_s[:, 2 * g + hh, :], start=True, stop=True),
                         reads=[kcb[s], b_qc], writes=[bb[0]])
                P.op(ACT, lambda e, k=k: e.activation(out=pe2[k], in_=banks[0][:, 0:256], func=AF.Exp, scale=ATT_SCALE),
                     reads=[bb[0]], writes=[pe2b[k]])
                P.op(DVE, lambda e, k=k, di=di: e.tensor_tensor(
                    out=pm2[k].rearrange("p (a b) -> p a b", b=128), in0=pe2[k].rearrange("p (a b) -> p a b", b=128),
                    in1=dmask[:, di, :].unsqueeze(1).to_broadcast([128, 2, 128]), op=ALU.mult),
                    reads=[pe2b[k], self.b_cst], writes=[pm2b[k]])
                for hh in range(2):
                    idx6 = 2 * g + hh
                    bk = 2 + idx6 // 3
                    col = (idx6 % 3) * 128
                    st_ = not started[bk]
                    started[bk] = True
                    P.op(PE, lambda e, s=s, k=k, hh=hh, bk=bk, col=col, st_=st_, idx6=idx6, n=n: e.matmul(
                        banks[bk][:, col:col + 128], vc_s[s][:, hh * 128:(hh + 1) * 128], pm2[k][:, hh * 128:(hh + 1) * 128],
                        start=st_, stop=(last_of_region[idx6] == n), skip_group_check=True),
                        reads=[vcb[s], pm2b[k]], writes=[bb[bk]])
                P.op(PE, lambda e, k=k, n=n: e.matmul(banks[4][:, 0:256], self.ones, pm2[k], start=(n == 0), stop=(n == nsteps - 1)),
                     reads=[self.b_cst, pm2b[k]], writes=[bb[4]])
            P.op(DVE, lambda e: e.reciprocal(out=rden[:, 0:256], in_=banks[4][:, 0:256]), reads=[bb[4]], writes=[b_rden])
            for idx6 in range(6):
                bk = 2 + idx6 // 3
                col = (idx6 % 3) * 128
                hh = idx6 % 2
                P.op(DVE, lambda e, idx6=idx6, bk=bk, col=col, hh=hh, tsl=tsl: e.tensor_tensor(
                    out=yT[:, 10 + idx6, tsl], in0=banks[bk][:, col:col + 128], in1=rden[:, hh * 128:(hh + 1) * 128], op=ALU.mult),
                    reads=[bb[bk], b_rden], writes=[self.yb[i]])

    def outproj(self, l):
        P, A = self.P, self.A
        banks, bb = self.banks, self.bb
        yT = self.yT
        xT = self.xT
        NW = 3
        A.reset(self.att_end)
        wo = [A.take([KC, 128], BF16) for _ in range(NW)]
        wob = [Buf("wo%d" % i) for i in range(NW)]
        wl = self.w_outp[l if self.mode == "F" else 0].rearrange("(kc p) n -> p kc n", p=128)
        for dc in range(KC):
            s = dc % NW
            P.dma(POOL, wo[s], wl[:, :, dc * 128:(dc + 1) * 128], reads=[self.wbuf[("w_out", l if self.mode == "F" else 0)]], writes=[wob[s]])
            bk = [self.nb(), self.nb()]
            for kc in range(KC):
                for tt in range(2):
                    P.op(PE, lambda e, s=s, kc=kc, tt=tt, bk=bk: e.matmul(
                        banks[bk[tt]], wo[s][:, kc, :], yT[:, kc, tt * 512:(tt + 1) * 512], start=(kc == 0), stop=(kc == KC - 1)),
                        reads=[wob[s]] + self.yb[tt * 4:(tt + 1) * 4], writes=[bb[bk[tt]]])
            for tt in range(2):
                P.op(DVE, lambda e, dc=dc, tt=tt, bk=bk: e.tensor_tensor(
                    out=xT[:, dc, tt * 512:(tt + 1) * 512], in0=banks[bk[tt]], in1=xT[:, dc, tt * 512:(tt + 1) * 512], op=ALU.add),
                    reads=[bb[bk[tt]], self.xb[dc]], writes=[self.xb[dc]])

    def final_norm_store(self):
        P, A = self.P, self.A
        xT = self.xT
        A.reset(KC * T * 4)
        ob = [A.take([T], F32) for _ in range(2)]
        obb = [Buf("ob0"), Buf("ob1")]

        def norm_out(c, rstd, rb, g):
            s = c % 2
            P.op(DVE, lambda e: e.scalar_tensor_tensor(out=ob[s], in0=xT[:, c, :], scalar=g[:, c:c + 1], in1=rstd,
                                                      op0=ALU.mult, op1=ALU.mult),
                 reads=[self.xb[c], rb, self.b_cst], writes=[obb[s]])
            P.dma(SP, self.x_out[c], ob[s], reads=[obb[s]])
        self.rmsnorm(self.gain(12), norm_out)

    def build(self, last=False):
        P = self.P
        mode = self.mode
        if mode == "A":
            self.carve_x()
            self.load_x(self.x_in)
            if "noffn" not in DEBUG:
                self.ffn(self.w_ffn1, 0, 0, self.n_ffn1)
            P.barrier()
            if "noinproj" not in DEBUG:
                self.inproj(0)
            self.store_x(self.x_d, self.b_xd)
        elif mode == "B":
            self.attention(0)
            P.barrier()
            self.carve_x()
            self.load_x(self.x_d)
            self.outproj(0)
            P.barrier()
            if "noffn2" not in DEBUG:
                self.ffn(self.w_ffn2, 0, 2, self.n_ffn2)
                P.barrier()
            if last:
                self.final_norm_store()
            else:
                self.store_x(self.x_out)
        else:
            self.carve_x()
            self.load_x(self.x_in)
            for l in range(DEPTH):
                self.ffn(self.w_ffn1, l, l * 3 + 0, self.n_ffn1)
                P.barrier()
                self.inproj(l)
                self.store_x(self.x_d, self.b_xd)
                P.op(POOL, lambda e: e.collective_compute("AllGather", ALU.bypass, replica_groups=[[0, 1, 2, 3], [4, 5, 6, 7]],
                                                         ins=[self.xchg.opt()], outs=[self.gath.opt()]),
                     reads=[self.b_xchg], writes=[self.b_gath], cc=True)
                P.barrier()
                self.attention(l)
                P.barrier()
                self.carve_x()
                self.load_x(self.x_d)
                self.outproj(l)
                P.barrier()
                self.ffn(self.w_ffn2, l, l * 3 + 2, self.n_ffn2)
                P.barrier()
            self.final_norm_store()
        P.barrier()
        P.emit()
        P.close()
        return self.nc


def _consts(core, norms, conv_w):
    r = core % 4
    cbm = np.zeros((128, NCB), dtype=np.float32)
    cbm[:, CB_ONES:CB_ONES + 128] = 1.0
    cbm[:, CB_ID:CB_ID + 128] = np.eye(128, dtype=np.float32)
    Rh = np.zeros((128, 128), dtype=np.float32)
    for m in range(64):
        Rh[m + 64, m] = -1.0
        Rh[m, m + 64] = 1.0
    Ri = np.zeros((128, 128), dtype=np.float32)
    for b0 in (0, 64):
        for m in range(32):
            Ri[b0 + m + 32, b0 + m] = -1.0
            Ri[b0 + m, b0 + m + 32] = 1.0
    cbm[:, CB_RH:CB_RH + 128] = Rh
    cbm[:, CB_RI:CB_RI + 128] = Ri
    s_idx = np.arange(128)[:, None]
    q_idx = np.arange(128)[None, :]
    for di, (g, kp) in enumerate(DIL_LIST):
        delta = r + DIL_X[g] - kp
        dist = 128 * delta + (q_idx - s_idx)
        ok = (dist >= 0) & (dist <= DIL_WIN[g]) & (dist % DIL_DIL[g] == 0)
        cbm[:, CB_DIL + di * 128:CB_DIL + (di + 1) * 128] = ok.astype(np.float32)
    cfm = np.zeros((128, NCF), dtype=np.float32)
    for n in range(13):
        cfm[:, CF_GAIN + n * 16:CF_GAIN + (n + 1) * 16] = norms[n].reshape(KC, 128).T
    for l in range(DEPTH):
        for j in range(3):
            cfm[:, CF_CONV + (l * 3 + j) * 4:CF_CONV + (l * 3 + j) * 4 + 4] = conv_w[l, j].reshape(4, 128).T
    inv_h = (1.0 / (np.float32(10000.0) ** (np.arange(0, 128, 2, dtype=np.float32) / np.float32(128)))).astype(np.float32)
    inv_i = (1.0 / (np.float32(10000.0) ** (np.arange(0, 64, 2, dtype=np.float32) / np.float32(64)))).astype(np.float32)
    p = np.arange(128)
    cfm[:, CF_INV] = inv_h[p % 64]
    cfm[:, CF_INV + 1] = inv_i[p % 32]
    cfm[:, CF_SEL + r] = 1.0
    cfm[:, CF_PW2:CF_PW2 + NIT] = (0.5 ** (np.arange(NIT) + 1)).astype(np.float32)[None, :]
    dm = np.zeros((128, 512), dtype=np.float32)
    for j in range(4):
        blk = dm[:, j * 128:(j + 1) * 128]
        if j > r:
            blk[:] = NEG
        elif j == r:
            blk[:] = np.where(np.arange(128)[None, :] <= np.arange(128)[:, None], 0.0, NEG)
    cfm[:, CF_DSA:CF_DSA + 512] = dm
    return cbm.astype(NPBF), cfm


def _core_tokens(core):
    r = core % 4
    idx = np.concatenate([np.arange((4 * i + r) * 128, (4 * i + r + 1) * 128) for i in range(NSLOT)])
    return core // 4, idx


_PROGS = {}


def _get_prog(mode, last=False):
    key = (mode, last)
    if key not in _PROGS:
        _PROGS[key] = Builder(mode).build(last=last)
    return _PROGS[key]


FUSED = False


def kernel(x, positions, norm_ffn1, ffn1_gate, ffn1_up, ffn1_down, norm_mix, w_in, conv_w, w_out,
           norm_ffn2, ffn2_gate, ffn2_up, ffn2_down, norm_final):
    x = np.asarray(x, dtype=np.float32)
    positions = np.asarray(positions)
    norms = []
    for l in range(DEPTH):
        norms += [np.asarray(norm_ffn1[l]), np.asarray(norm_mix[l]), np.asarray(norm_ffn2[l])]
    norms.append(np.asarray(norm_final))
    conv_w = np.asarray(conv_w, dtype=np.float32)
    cores = list(range(NCORE))
    xT, pos, cbs, cfs = [], [], [], []
    for c in cores:
        b, idx = _core_tokens(c)
        xT.append(np.ascontiguousarray(x[b, idx, :].T.reshape(KC, 128, T)))
        pos.append(np.ascontiguousarray(positions[b, idx].astype(np.int32).reshape(1, T)))
        cbm, cfm = _consts(c, norms, conv_w)
        cbs.append(cbm)
        cfs.append(cfm)
    def wsh(w, c, l=None):
        w = np.asarray(w)
        if l is not None:
            w = w[l:l + 1]
        if not WSHARD:
            return w
        n = w.shape[1] // NCORE
        return np.ascontiguousarray(w[:, c * n:(c + 1) * n])
    if FUSED:
        nc = _get_prog("F")
        in_maps = []
        for c in cores:
            in_maps.append({"cst_bf": cbs[c], "cst_f32": cfs[c], "x_in": xT[c], "pos": pos[c],
                            "ffn1_gate": wsh(ffn1_gate, c), "ffn1_up": wsh(ffn1_up, c), "ffn1_down": wsh(ffn1_down, c),
                            "w_in": wsh(w_in, c), "ffn2_gate": wsh(ffn2_gate, c), "ffn2_up": wsh(ffn2_up, c),
                            "ffn2_down": wsh(ffn2_down, c), "w_out": wsh(w_out, c)})
        res = run_bass_kernel_spmd(nc, in_maps, core_ids=cores)
        outs = [res.results[c]["x_out"] for c in cores]
    else:
        cur = xT
        outs = None
        for l in range(DEPTH):
            cf_l = []
            for c in cores:
                m = cfs[c].copy()
                for n in range(3):
                    m[:, CF_GAIN + n * 16:CF_GAIN + (n + 1) * 16] = cfs[c][:, CF_GAIN + (l * 3 + n) * 16:CF_GAIN + (l * 3 + n + 1) * 16]
                m[:, CF_CONV:CF_CONV + 12] = cfs[c][:, CF_CONV + l * 12:CF_CONV + (l + 1) * 12]
                cf_l.append(m)
            ncA = _get_prog("A")
            in_maps = [{"cst_bf": cbs[c], "cst_f32": cf_l[c], "x_in": cur[c], "pos": pos[c],
                        "ffn1_gate": wsh(ffn1_gate, c, l), "ffn1_up": wsh(ffn1_up, c, l), "ffn1_down": wsh(ffn1_down, c, l),
                        "w_in": wsh(w_in, c, l)} for c in cores]
            ra = run_bass_kernel_spmd(ncA, in_maps, core_ids=cores).results
            last = l == DEPTH - 1
            ncB = _get_prog("B", last)
            in_maps = []
            for c in cores:
                g0 = (c // 4) * 4
                gath = np.concatenate([ra[g0 + k]["xchg"] for k in range(4)], axis=0)
                in_maps.append({"cst_bf": cbs[c], "cst_f32": cf_l[c], "gath": gath,
                                "x_d": ra[c]["x_d"], "qb_d": ra[c]["qb_d"], "qi_d": ra[c]["qi_d"], "qc_d": ra[c]["qc_d"],
                                "wi_d": ra[c]["wi_d"], "u_d": ra[c]["u_d"], "gb_d": ra[c]["gb_d"],
                                "ffn2_gate": wsh(ffn2_gate, c, l), "ffn2_up": wsh(ffn2_up, c, l), "ffn2_down": wsh(ffn2_down, c, l),
                                "w_out": wsh(w_out, c, l)})
            rb = run_bass_kernel_spmd(ncB, in_maps, core_ids=cores).results
            cur = [rb[c]["x_out"] for c in cores]
        outs = cur
    out = np.empty((2, SEQ, D), dtype=np.float32)
    for c in cores:
        b, idx = _core_tokens(c)
        out[b, idx, :] = outs[c].reshape(D, T).T
    return out
```

```python
import contextlib
import numpy as np
import ml_dtypes
import concourse.bass as bass
import concourse.mybir as mybir
from concourse.bass_utils import run_bass_kernel_spmd

F32 = mybir.dt.float32
BF16 = mybir.dt.bfloat16
I32 = mybir.dt.int32
AF = mybir.ActivationFunctionType
ALU = mybir.AluOpType
AX = mybir.AxisListType
NPBF = ml_dtypes.bfloat16

PE, ACT, DVE, POOL, SP = "tensor", "scalar", "vector", "gpsimd", "sync"
COMPUTE = (PE, ACT, DVE, POOL)

D = 2048
DFF = 5632
DIN = 5968
DEPTH = 4
T = 1024
KC = 16
NSLOT = 8
NJ = 44
G = 11
SEQ = 4096
NCORE = 8
XROWS = 1864
R_KB, R_KI, R_KC, R_VB, R_VC, R_UH = 0, 128, 192, 960, 1088, 1856
NIT = 26
TOPK = 256
ATT_SCALE = 128 ** -0.5
DIL_X = (1, 4, 16)
DIL_WIN = (128, 512, 2048)
DIL_DIL = (1, 4, 16)
DIL_LIST = [(g, kp) for g in range(3) for kp in range(DIL_X[g] + 4)]
NDIL = len(DIL_LIST)
C_H, C_GB, C_GC, C_QB, C_KB, C_VB, C_QI, C_KI, C_WI, C_QC, C_KC, C_VC = (
    0, 512, 1024, 1536, 2304, 2432, 2560, 3584, 3648, 3664, 4432, 5200)
CB_ONES, CB_ID, CB_RH, CB_RI, CB_DIL = 0, 128, 256, 384, 512
NCB = 512 + NDIL * 128
CF_GAIN, CF_CONV, CF_INV, CF_SEL, CF_PW2, CF_DSA = 0, 208, 256, 258, 262, 262 + NIT
NCF = CF_DSA + 512
NEG = -3.0e38
WSHARD = False
import os
DEBUG = os.environ.get('K_DEBUG', '').split(',')


class Buf:
    __slots__ = ("name", "writer", "readers", "excl")

    def __init__(self, name="", excl=False):
        self.name = name
        self.writer = None
        self.readers = []
        self.excl = excl


class Op:
    __slots__ = ("stream", "fn", "deps", "is_dma", "sidx", "need_inc", "tok",
                 "dsem", "dval", "waits", "prewait", "barred", "dkey", "is_cc")

    def __init__(self, stream, fn, is_dma, is_cc=False):
        self.stream = stream
        self.fn = fn
        self.is_dma = is_dma
        self.is_cc = is_cc
        self.deps = []
        self.need_inc = False
        self.tok = None
        self.waits = []
        self.prewait = None
        self.barred = False
        self.dkey = None


class Prog:
    NDSEM = 12

    def __init__(self, nc):
        self.nc = nc
        self.ops = {s: [] for s in (PE, ACT, DVE, POOL, SP)}
        self.all_ops = []
        self.stack = contextlib.ExitStack()

    def sbuf(self, name, shape, dt):
        return self.stack.enter_context(self.nc.sbuf_tensor(name, list(shape), dt))[:]

    def psum(self, name, shape, dt):
        return self.stack.enter_context(self.nc.psum_tensor(name, list(shape), dt))[:]

    def op(self, stream, fn, reads=(), writes=(), dma=False, cc=False):
        o = Op(stream, fn, dma or cc, cc)
        if cc:
            if not hasattr(self, "cc_chain"):
                self.cc_chain = Buf("cc_chain")
            writes = list(writes) + [self.cc_chain]
        deps = set()
        ex = [b for b in reads if b.excl]
        if ex:
            reads = [b for b in reads if not b.excl]
            writes = list(writes) + [b for b in ex if b not in writes]
        for b in reads:
            if b.writer is not None:
                deps.add(b.writer)
        for b in writes:
            if b.writer is not None:
                deps.add(b.writer)
            for r in b.readers:
                deps.add(r)
        o.deps = list(deps)
        for b in reads:
            b.readers.append(o)
        for b in writes:
            b.writer = o
            b.readers = []
        o.sidx = len(self.ops[stream])
        self.ops[stream].append(o)
        self.all_ops.append(o)
        return o

    def dma(self, stream, out, in_, reads=(), writes=()):
        return self.op(stream, lambda e: e.dma_start(out=out, in_=in_), reads=reads, writes=writes, dma=True)

    def barrier(self):
        lasts = [self.ops[s][-1] for s in self.ops if self.ops[s]]
        dmas = [o for o in self.all_ops if o.is_dma and not o.barred and not o.is_cc]
        for o in dmas:
            o.barred = True
        for s in self.ops:
            o = Op(s, None, False)
            o.deps = list(lasts) + list(dmas)
            o.sidx = len(self.ops[s])
            self.ops[s].append(o)
            self.all_ops.append(o)

    def emit(self):
        nc = self.nc
        st = self.stack
        sems = {s: st.enter_context(nc.semaphore("s_" + s)) for s in COMPUTE}
        dsems = {q: [st.enter_context(nc.semaphore("d_%s_%d" % (q, i))) for i in range(self.NDSEM)]
                 for q in (SP, POOL)}
        dsems["cc"] = [st.enter_context(nc.semaphore("cc_%d" % i)) for i in range(self.NDSEM)]
        dcount = {q: [0] * self.NDSEM for q in dsems}
        dnext = {q: 0 for q in dsems}
        for s, lst in self.ops.items():
            seen_c = {}
            seen_d = {}
            for o in lst:
                if o.is_dma:
                    q = "cc" if o.is_cc else s
                    inc = 1 if o.is_cc else 16
                    j = dnext[q]
                    dnext[q] = (j + 1) % self.NDSEM
                    prev = dcount[q][j]
                    if prev > 0 and seen_d.get((q, j), 0) < inc * prev:
                        o.prewait = (dsems[q][j], inc * prev)
                        seen_d[(q, j)] = inc * prev
                    dcount[q][j] = prev + 1
                    o.dsem = dsems[q][j]
                    o.dval = inc * (prev + 1)
                    o.dkey = (q, j)
                for d in o.deps:
                    if d.is_dma or d.fn is None:
                        continue
                    if d.stream == s and s == PE:
                        continue
                    if seen_c.get(d.stream, -1) >= d.sidx:
                        continue
                    seen_c[d.stream] = d.sidx
                    d.need_inc = True
                    o.waits.append(("c", d))
        for s, lst in self.ops.items():
            seen_d = {}
            for o in lst:
                if o.prewait is not None:
                    seen_d[o.dkey] = max(seen_d.get(o.dkey, 0), o.prewait[1])
                for d in o.deps:
                    if not d.is_dma:
                        continue
                    if seen_d.get(d.dkey, 0) >= d.dval:
                        continue
                    seen_d[d.dkey] = d.dval
                    o.waits.append(("d", d))
        for s in COMPUTE:
            c = 0
            for o in self.ops[s]:
                if o.fn is not None and not o.is_dma and o.need_inc:
                    c += 1
                    o.tok = c
        self.stats = {s: len(l) for s, l in self.ops.items()}
        block = st.enter_context(nc.Block())

        def run(stream):
            def body(eng):
                for o in self.ops[stream]:
                    if o.prewait is not None:
                        eng.wait_ge(o.prewait[0], o.prewait[1])
                    for kind, d in o.waits:
                        if kind == "c":
                            eng.wait_ge(sems[d.stream], d.tok)
                        else:
                            eng.wait_ge(d.dsem, d.dval)
                    if o.fn is None:
                        continue
                    ins = o.fn(eng)
                    if o.is_cc:
                        ins.then_inc(o.dsem)
                    elif o.is_dma:
                        ins.then_inc(o.dsem, 16)
                    elif o.need_inc:
                        ins.then_inc(sems[stream], 1)
            return body

        block.tensor(run(PE))
        block.vector(run(DVE))
        block.scalar(run(ACT))
        block.gpsimd(run(POOL))
        block.sync(run(SP))

    def close(self):
        self.stack.close()


class Arena:
    def __init__(self, P, nbytes):
        self.t = P.sbuf("arena", [128, nbytes // 2], BF16)
        self.nbytes = nbytes
        self.off = 0

    def reset(self, off=0):
        self.off = off

    def take(self, free_shape, dt):
        sz = 2 if dt == BF16 else 4
        n = int(np.prod(free_shape))
        nb = n * sz
        nb_al = (nb + 63) // 64 * 64
        assert self.off + nb_al <= self.nbytes, ("arena overflow", self.off, nb_al, self.nbytes)
        ap = self.t[:, self.off // 2:(self.off + nb) // 2]
        if dt != BF16:
            ap = ap.bitcast(dt)
        self.off += nb_al
        if len(free_shape) == 2:
            ap = ap.rearrange("p (a b) -> p a b", b=free_shape[1])
        elif len(free_shape) == 3:
            ap = ap.rearrange("p (a b c) -> p a b c", b=free_shape[1], c=free_shape[2])
        elif len(free_shape) == 4:
            ap = ap.rearrange("p (a b c d) -> p a b c d", b=free_shape[1], c=free_shape[2], d=free_shape[3])
        return ap


class Builder:
    def __init__(self, mode, layer=0):
        self.mode = mode
        self.layer = layer
        nc = bass.Bass("TRN2", target_bir_lowering=False)
        self.nc = nc
        self.P = P = Prog(nc)
        fused = mode == "F"
        nl = DEPTH if fused else 1

        def ext_in(name, shape, dt):
            return nc.dram_tensor(name, list(shape), dt, kind="ExternalInput").ap()

        def ext_out(name, shape, dt):
            return nc.dram_tensor(name, list(shape), dt, kind="ExternalOutput").ap()

        def scratch(name, shape, dt, produced_by):
            if fused:
                return nc.dram_tensor(name, list(shape), dt).ap()
            if produced_by == mode:
                return ext_out(name, shape, dt)
            return ext_in(name, shape, dt)

        self.wbuf = {}

        def weight(name, rows, cols):
            if not WSHARD:
                ap = ext_in(name, [nl, rows, cols], F32)
                for l in range(nl):
                    self.wbuf[(name, l)] = Buf(name)
                return ap
            shard = ext_in(name, [nl, rows // NCORE, cols], F32)
            bounce = nc.dram_tensor(name + "_bnc", [nl, rows // NCORE, cols], F32).ap()
            full = nc.dram_tensor(name + "_full", [nl, rows, cols], F32).ap()
            for l in range(nl):
                bb_ = Buf(name + "_b%d" % l)
                fb = Buf(name + "_f%d" % l)
                self.wbuf[(name, l)] = fb
                self.wgather.append((l, name, shard[l], bounce[l], full[l], bb_, fb))
            return full
        self.wgather = []
        self.cb_in = ext_in("cst_bf", [128, NCB], BF16)
        self.cf_in = ext_in("cst_f32", [128, NCF], F32)
        if mode in ("A", "F"):
            self.x_in = ext_in("x_in", [KC, 128, T], F32)
            self.pos_in = ext_in("pos", [1, T], I32)
            if "noffn" not in DEBUG:
                self.w_ffn1 = [weight("ffn1_gate", D, DFF), weight("ffn1_up", D, DFF), weight("ffn1_down", DFF, D)]
                self.n_ffn1 = ["ffn1_gate", "ffn1_up", "ffn1_down"]
            self.w_inp = weight("w_in", D, DIN)
        if mode in ("B", "F"):
            self.w_outp = weight("w_out", D, D)
            if "noffn2" not in DEBUG:
                self.w_ffn2 = [weight("ffn2_gate", D, DFF), weight("ffn2_up", D, DFF), weight("ffn2_down", DFF, D)]
                self.n_ffn2 = ["ffn2_gate", "ffn2_up", "ffn2_down"]
            self.x_out = ext_out("x_out", [KC, 128, T], F32)
        self.x_d = scratch("x_d", [KC, 128, T], F32, "A")
        self.qb_d = scratch("qb_d", [6, 128, T], BF16, "A")
        self.qi_d = scratch("qi_d", [8, 128, T], BF16, "A")
        self.qc_d = scratch("qc_d", [6, 128, T], BF16, "A")
        self.wi_d = scratch("wi_d", [NSLOT, 128, 16], F32, "A")
        self.u_d = scratch("u_d", [4, 128, T], BF16, "A")
        self.gb_d = scratch("gb_d", [4, 128, T], BF16, "A")
        if mode in ("A", "F"):
            self.xchg = scratch("xchg", [XROWS, T], BF16, "A")
        if fused:
            self.gath = nc.dram_tensor("gath", [4 * XROWS, T], BF16).ap()
        elif mode == "B":
            self.gath = ext_in("gath", [4 * XROWS, T], BF16)
        self.b_q = Buf("qside")
        self.b_xchg = Buf("xchg")
        self.b_gath = Buf("gath")

        self.cb = P.sbuf("cb", [128, NCB], BF16)
        self.cf = P.sbuf("cf", [128, NCF], F32)
        self.b_cst = Buf("cst")
        self.eps = P.sbuf("epsc", [128, 1], F32)
        self.banks = [P.psum("bank%d" % i, [128, 512], F32) for i in range(8)]
        self.bb = [Buf("bank%d" % i, excl=True) for i in range(8)]
        self.bank_rr = 0
        remaining = int(nc.sbuf_bytes_remaining)
        per_part = remaining // 128 if remaining > 400 * 1024 else remaining
        self.arena_bytes = (per_part - 1024) // 64 * 64
        self.A = Arena(P, self.arena_bytes)
        P.dma(SP, self.cb, self.cb_in, writes=[self.b_cst])
        P.dma(SP, self.cf, self.cf_in, writes=[self.b_cst])
        P.op(DVE, lambda e: e.memset(self.eps, 1e-6), writes=[self.b_cst])
        if mode == "F":
            key = lambda t: (t[0], {"ffn1_gate": 0, "ffn1_up": 1, "ffn1_down": 2, "w_in": 3, "w_out": 4,
                                    "ffn2_gate": 5, "ffn2_up": 6, "ffn2_down": 7}[t[1]])
        else:
            key = lambda t: {"ffn1_gate": 0, "ffn1_up": 1, "ffn1_down": 2, "w_in": 3, "w_out": 4,
                             "ffn2_gate": 5, "ffn2_up": 6, "ffn2_down": 7}[t[1]]
        self.wgather.sort(key=key)
        for _ in range(4):
            self.issue_gather()
        self.ones = self.cb[:, CB_ONES:CB_ONES + 128]
        self.ident = self.cb[:, CB_ID:CB_ID + 128]
        self.xb = [Buf("x%d" % c) for c in range(KC)]
        self.A.reset(0)
        self.xT = self.A.take([KC, T], F32)
        self.b_xd = [Buf("x_d%d" % c) for c in range(KC)]

    def issue_gather(self):
        if not self.wgather:
            return
        (l, name, shard, bounce, full, bb_, fb) = self.wgather.pop(0)
        P = self.P
        P.dma(SP, bounce, shard, writes=[bb_])
        P.op(POOL, lambda e: e.collective_compute("AllGather", ALU.bypass, replica_groups=[list(range(NCORE))],
                                                 ins=[bounce.opt()], outs=[full.opt()]),
             reads=[bb_], writes=[fb], cc=True)

    def nb(self):
        i = self.bank_rr
        self.bank_rr = (i + 1) % 8
        return i

    def gain(self, n):
        return self.cf[:, CF_GAIN + n * 16:CF_GAIN + (n + 1) * 16]

    def carve_x(self):
        pass

    def load_x(self, src):
        for c in range(KC):
            self.P.dma(SP, self.xT[:, c, :], src[c], reads=[self.b_xd[c]], writes=[self.xb[c]])

    def store_x(self, dst, buf=None):
        for c in range(KC):
            self.P.dma(SP, dst[c], self.xT[:, c, :], reads=[self.xb[c]], writes=[buf[c]] if buf is not None else [])

    def rmsnorm(self, gain_ap, out_fn):
        P, A = self.P, self.A
        xT = self.xT
        sq = [A.take([T], BF16) for _ in range(2)]
        sqb = [Buf("sq0"), Buf("sq1")]
        rstd = A.take([T], F32)
        rstd2 = A.take([T], F32)
        rb, rb2 = Buf("rstd"), Buf("rstd2")
        b0, b1 = self.nb(), self.nb()
        bk = (b0, b1)
        for c in range(KC):
            s = c % 2
            P.op(ACT, lambda e, c=c, s=s: e.activation(out=sq[s], in_=xT[:, c, :], func=AF.Square),
                 reads=[self.xb[c]], writes=[sqb[s]])
            for tt in range(2):
                P.op(PE, lambda e, c=c, s=s, tt=tt: e.matmul(self.banks[bk[tt]], self.ones, sq[s][:, tt * 512:(tt + 1) * 512],
                                                            start=(c == 0), stop=(c == KC - 1)),
                     reads=[sqb[s], self.b_cst], writes=[self.bb[bk[tt]]])
        for tt in range(2):
            P.op(ACT, lambda e, tt=tt: e.activation(out=rstd[:, tt * 512:(tt + 1) * 512], in_=self.banks[bk[tt]],
                                                   func=AF.Sqrt, scale=1.0 / D, bias=self.eps[:, 0:1]),
                 reads=[self.bb[bk[tt]], self.b_cst], writes=[rb])
        P.op(DVE, lambda e: e.reciprocal(out=rstd2, in_=rstd), reads=[rb], writes=[rb2])
        for c in range(KC):
            out_fn(c, rstd2, rb2, gain_ap)

    def ffn(self, wts, l, gain_n, names=None):
        P, A = self.P, self.A
        xT = self.xT
        A.reset(KC * T * 4)
        xn = A.take([KC, T], BF16)
        xnb = [Buf("xn%d" % c) for c in range(KC)]
        mark = A.off

        def norm_out(c, rstd, rb, g):
            P.op(DVE, lambda e: e.scalar_tensor_tensor(out=xn[:, c, :], in0=xT[:, c, :], scalar=g[:, c:c + 1], in1=rstd,
                                                      op0=ALU.mult, op1=ALU.mult),
                 reads=[self.xb[c], rb, self.b_cst], writes=[xnb[c]])
        self.rmsnorm(self.gain(gain_n), norm_out)
        P.barrier()
        A.reset(mark)
        hT = A.take([G, T], BF16)
        hb = [Buf("h%d" % j) for j in range(G)]
        NGU = 3
        gu = [(A.take([KC, 128], BF16), A.take([KC, 128], BF16)) for _ in range(NGU)]
        gub = [(Buf("wg%d" % i), Buf("wu%d" % i)) for i in range(NGU)]
        ND, QW = 2, 512
        dwt = [A.take([G, QW], BF16) for _ in range(ND)]
        dwb = [Buf("wd%d" % i) for i in range(ND)]
        stg = [A.take([512], BF16) for _ in range(2)]
        stb = [Buf("st0"), Buf("st1")]
        wgv = wts[0][l].rearrange("(kc p) n -> p kc n", p=128)
        wuv = wts[1][l].rearrange("(kc p) n -> p kc n", p=128)
        wdv = wts[2][l].rearrange("(j p) n -> p j n", p=128)
        gucnt = dcnt = stc = 0
        banks, bb = self.banks, self.bb
        for g in range(NJ // G):
            for jj in range(G):
                j = g * G + jj
                s = gucnt % NGU
                gucnt += 1
                if jj == 5:
                    self.issue_gather()
                P.dma(POOL, gu[s][0], wgv[:, :, j * 128:(j + 1) * 128], reads=[self.wbuf[(names[0], l)]], writes=[gub[s][0]])
                P.dma(POOL, gu[s][1], wuv[:, :, j * 128:(j + 1) * 128], reads=[self.wbuf[(names[1], l)]], writes=[gub[s][1]])
                bg = [self.nb(), self.nb()]
                bu = [self.nb(), self.nb()]
                for which, bk in ((0, bg), (1, bu)):
                    for kc in range(KC):
                        for tt in range(2):
                            P.op(PE, lambda e, s=s, which=which, kc=kc, tt=tt, bk=bk: e.matmul(
                                banks[bk[tt]], gu[s][which][:, kc, :], xn[:, kc, tt * 512:(tt + 1) * 512],
                                start=(kc == 0), stop=(kc == KC - 1)),
                                reads=[gub[s][which], xnb[kc]], writes=[bb[bk[tt]]])
                for tt in range(2):
                    k = stc % 2
                    stc += 1
                    P.op(ACT, lambda e, k=k, tt=tt, bg=bg: e.activation(out=stg[k], in_=banks[bg[tt]], func=AF.Silu),
                         reads=[bb[bg[tt]]], writes=[stb[k]])
                    P.op(DVE, lambda e, k=k, tt=tt, bu=bu, jj=jj: e.tensor_tensor(
                        out=hT[:, jj, tt * 512:(tt + 1) * 512], in0=stg[k], in1=banks[bu[tt]], op=ALU.mult),
                        reads=[stb[k], bb[bu[tt]]], writes=[hb[jj]])
            for q in range(D // QW):
                s = dcnt % ND
                dcnt += 1
                P.dma(POOL, dwt[s], wdv[:, g * G:(g + 1) * G, q * QW:(q + 1) * QW], reads=[self.wbuf[(names[2], l)]], writes=[dwb[s]])
                for dd in range(QW // 128):
                    dc = q * (QW // 128) + dd
                    bk = [self.nb(), self.nb()]
                    for jj in range(G):
                        for tt in range(2):
                            P.op(PE, lambda e, s=s, jj=jj, tt=tt, dd=dd, bk=bk: e.matmul(
                                banks[bk[tt]], dwt[s][:, jj, dd * 128:(dd + 1) * 128], hT[:, jj, tt * 512:(tt + 1) * 512],
                                start=(jj == 0), stop=(jj == G - 1)),
                                reads=[dwb[s], hb[jj]], writes=[bb[bk[tt]]])
                    for tt in range(2):
                        P.op(DVE, lambda e, dc=dc, tt=tt, bk=bk: e.scalar_tensor_tensor(
                            out=xT[:, dc, tt * 512:(tt + 1) * 512], in0=banks[bk[tt]], scalar=0.5,
                            in1=xT[:, dc, tt * 512:(tt + 1) * 512], op0=ALU.mult, op1=ALU.add),
                            reads=[bb[bk[tt]], self.xb[dc]], writes=[self.xb[dc]])

    def inproj(self, l):
        P, A = self.P, self.A
        xT, banks, bb = self.xT, self.banks, self.bb
        cf = self.cf
        A.reset(KC * T * 4)
        xn = A.take([KC, T], BF16)
        xnb = [Buf("xn%d" % c) for c in range(KC)]
        tabs = [A.take([T], F32) for _ in range(4)]
        tabb = Buf("tabs")
        mark = A.off
        posi = A.take([T], I32)
        posf = A.take([T], F32)
        ang = A.take([T], F32)
        kf = A.take([T], F32)
        ki = A.take([T], I32)
        y1 = A.take([T], F32)
        y2 = A.take([T], F32)
        tb = [Buf("tt%d" % i) for i in range(7)]
        P.dma(SP, posi, self.pos_in.partition_broadcast(128), writes=[tb[0]])
        P.op(DVE, lambda e: e.tensor_copy(out=posf, in_=posi), reads=[tb[0]], writes=[tb[1]])
        C1 = 6.28125
        C2 = float(2 * np.pi - 6.28125)
        for ti, (invcol, shift) in enumerate(((0, np.pi / 2), (0, 0.0), (1, np.pi / 2), (1, 0.0))):
            P.op(DVE, lambda e, invcol=invcol, shift=shift: e.tensor_scalar(
                out=ang, in0=posf, scalar1=cf[:, CF_INV + invcol:CF_INV + invcol + 1], scalar2=float(shift),
                op0=ALU.mult, op1=ALU.add), reads=[tb[1], self.b_cst], writes=[tb[2]])
            P.op(DVE, lambda e: e.tensor_scalar(out=kf, in0=ang, scalar1=float(1 / (2 * np.pi)), scalar2=None, op0=ALU.mult),
                 reads=[tb[2]], writes=[tb[3]])
            P.op(DVE, lambda e: e.tensor_copy(out=ki, in_=kf), reads=[tb[3]], writes=[tb[4]])
            P.op(DVE, lambda e: e.tensor_copy(out=kf, in_=ki), reads=[tb[4]], writes=[tb[3]])
            P.op(DVE, lambda e: e.scalar_tensor_tensor(out=y1, in0=kf, scalar=-C1, in1=ang, op0=ALU.mult, op1=ALU.add),
                 reads=[tb[3], tb[2]], writes=[tb[5]])
            P.op(DVE, lambda e: e.scalar_tensor_tensor(out=y2, in0=kf, scalar=-C2, in1=y1, op0=ALU.mult, op1=ALU.add),
                 reads=[tb[3], tb[5]], writes=[tb[6]])
            P.op(ACT, lambda e, ti=ti: e.activation(out=tabs[ti], in_=y2, func=AF.Sin, scale=1.0 - 1e-6),
                 reads=[tb[6]], writes=[tabb])
        P.barrier()
        A.reset(mark)
        if "tables_only" in DEBUG:
            return

        def norm_out(c, rstd, rb, g):
            P.op(DVE, lambda e: e.scalar_tensor_tensor(out=xn[:, c, :], in0=xT[:, c, :], scalar=g[:, c:c + 1], in1=rstd,
                                                      op0=ALU.mult, op1=ALU.mult),
                 reads=[self.xb[c], rb, self.b_cst], writes=[xnb[c]])
        self.rmsnorm(self.gain(l * 3 + 1 if self.mode == "F" else 1), norm_out)
        NW = 3
        wch = [A.take([KC, 128], BF16) for _ in range(NW)]
        wchb = [Buf("wch%d" % i) for i in range(NW)]
        ost = [A.take([T], BF16) for _ in range(3)]
        ostb = [Buf("ost%d" % i) for i in range(3)]
        hs = [A.take([T], F32) for _ in range(2)]
        hsb = [Buf("hs0"), Buf("hs1")]
        q16 = [A.take([512], BF16) for _ in range(2)]
        q16b = [Buf("q16_0"), Buf("q16_1")]
        t1 = [A.take([512], F32) for _ in range(2)]
        t1b = [Buf("t1_0"), Buf("t1_1")]
        t2 = [A.take([512], F32) for _ in range(2)]
        t2b = [Buf("t2_0"), Buf("t2_1")]
        tst = [A.take([NSLOT, 128], BF16) for _ in range(2)]
        tstb = [Buf("tst0"), Buf("tst1")]
        wst = A.take([NSLOT, 16], F32)
        wstb = Buf("wst")
        wl = self.w_inp[l if self.mode == "F" else 0]
        wv = wl.rearrange("(kc p) n -> p kc n", p=128)
        b_win = self.wbuf[("w_in", l if self.mode == "F" else 0)]
        xchg = self.xchg
        cnt = {"w": 0, "o": 0, "r": 0, "h": 0, "t": 0}
        R_h = self.cb[:, CB_RH:CB_RH + 128]
        R_i = self.cb[:, CB_RI:CB_RI + 128]

        def fm_chunk(col, M, kind, dst, dst_buf, tab=None, hslot=None, extra=None):
            s = cnt["w"] % NW
            cnt["w"] += 1
            P.dma(POOL, wch[s][:, :, 0:M], wv[:, :, col:col + M], reads=[b_win], writes=[wchb[s]])
            bk = [self.nb(), self.nb()]
            for kc in range(KC):
                for tt in range(2):
                    P.op(PE, lambda e, s=s, kc=kc, tt=tt, bk=bk: e.matmul(
                        banks[bk[tt]][0:M, :], wch[s][:, kc, 0:M], xn[:, kc, tt * 512:(tt + 1) * 512],
                        start=(kc == 0), stop=(kc == KC - 1)),
                        reads=[wchb[s], xnb[kc]], writes=[bb[bk[tt]]])
            if kind == "h":
                for tt in range(2):
                    P.op(ACT, lambda e, tt=tt, bk=bk: e.activation(out=hs[hslot][:, tt * 512:(tt + 1) * 512],
                                                                 in_=banks[bk[tt]], func=AF.Copy),
                         reads=[bb[bk[tt]]], writes=[hsb[hslot]])
                return
            o = cnt["o"] % 3
            cnt["o"] += 1
            for tt in range(2):
                sl = slice(tt * 512, (tt + 1) * 512)
                if kind == "gc":
                    P.op(DVE, lambda e, tt=tt, bk=bk, sl=sl, o=o: e.tensor_tensor(
                        out=ost[o][:, sl], in0=hs[hslot][:, sl], in1=banks[bk[tt]], op=ALU.mult),
                        reads=[hsb[hslot], bb[bk[tt]]], writes=[ostb[o]])
                elif kind == "gb":
                    P.op(ACT, lambda e, tt=tt, bk=bk, sl=sl, o=o: e.activation(out=ost[o][:, sl], in_=banks[bk[tt]], func=AF.Copy),
                         reads=[bb[bk[tt]]], writes=[ostb[o]])
                else:
                    k = cnt["r"] % 2
                    cnt["r"] += 1
                    cosT, sinT, Rm = tab
                    rb_ = self.nb()
                    if "rope_nomm" in DEBUG:
                        rb_ = bk[tt]
                    else:
                        P.op(ACT, lambda e, k=k, tt=tt, bk=bk: e.activation(out=q16[k][0:M, :], in_=banks[bk[tt]][0:M, :], func=AF.Copy),
                             reads=[bb[bk[tt]]], writes=[q16b[k]])
                        P.op(PE, lambda e, k=k, rb_=rb_, Rm=Rm: e.matmul(banks[rb_][0:M, :], Rm[0:M, 0:M], q16[k][0:M, :],
                                                                       start=True, stop=True),
                             reads=[q16b[k], self.b_cst], writes=[bb[rb_]])
                    if "rope_nomul" in DEBUG:
                        P.op(DVE, lambda e, k=k, tt=tt, bk=bk: e.tensor_copy(out=t1[k][0:M, :], in_=banks[bk[tt]][0:M, :]),
                             reads=[bb[bk[tt]]], writes=[t1b[k]])
                        P.op(DVE, lambda e, k=k, rb_=rb_: e.tensor_copy(out=t2[k][0:M, :], in_=banks[rb_][0:M, :]),
                             reads=[bb[rb_]], writes=[t2b[k]])
                    else:
                        P.op(DVE, lambda e, k=k, tt=tt, bk=bk, sl=sl, cosT=cosT: e.tensor_tensor(
                            out=t1[k][0:M, :], in0=banks[bk[tt]][0:M, :], in1=cosT[0:M, sl], op=ALU.mult),
                            reads=[bb[bk[tt]], tabb], writes=[t1b[k]])
                        P.op(DVE, lambda e, k=k, rb_=rb_, sl=sl, sinT=sinT: e.tensor_tensor(
                            out=t2[k][0:M, :], in0=banks[rb_][0:M, :], in1=sinT[0:M, sl], op=ALU.mult),
                            reads=[bb[rb_], tabb], writes=[t2b[k]])
                    P.op(DVE if "ropeadd_dve" in DEBUG else POOL, lambda e, k=k, sl=sl, o=o: e.tensor_tensor(
                        out=ost[o][0:M, sl], in0=t1[k][0:M, :], in1=t2[k][0:M, :], op=ALU.add),
                        reads=[t1b[k], t2b[k]], writes=[ostb[o]])
            P.dma(SP, dst, ost[o][0:M, :], reads=[ostb[o]], writes=[dst_buf])
            if extra is not None:
                extra(o)

        rope_h = (tabs[0], tabs[1], R_h)
        rope_i = (tabs[2], tabs[3], R_i)
        if "norm_only" in DEBUG:
            return
        uh_flat = xchg[R_UH:R_UH + 8, :].rearrange("r (a b) -> (r a) b", b=16).rearrange("(c p) (i t) -> c p i t", p=128, t=2)
        for c in range(4):
            if "noconv" in DEBUG:
                break
            hslot = c % 2
            fm_chunk(C_H + c * 128, 128, "h", None, None, hslot=hslot)

            def uh_extra(o, c=c):
                P.dma(SP, uh_flat[c], ost[o].rearrange("p (i s) -> p i s", s=128)[:, :, 126:128],
                      reads=[ostb[o]], writes=[self.b_xchg])
            fm_chunk(C_GC + c * 128, 128, "gc", self.u_d[c], self.b_q, hslot=hslot, extra=uh_extra)
            fm_chunk(C_GB + c * 128, 128, "gb", self.gb_d[c], self.b_q)
        if "conv_only" in DEBUG:
            return
        for h in range(6):
            fm_chunk(C_QB + h * 128, 128, "rope", self.qb_d[h], self.b_q, tab=rope_h)
        if "qb_only" in DEBUG:
            return
        fm_chunk(C_KB, 128, "rope", xchg[R_KB:R_KB + 128, :], self.b_xchg, tab=rope_h)
        for c in range(8):
            fm_chunk(C_QI + c * 128, 128, "rope", self.qi_d[c], self.b_q, tab=rope_i)
        fm_chunk(C_KI, 64, "rope", xchg[R_KI:R_KI + 64, :], self.b_xchg, tab=rope_i)
        for h in range(6):
            fm_chunk(C_QC + h * 128, 128, "rope", self.qc_d[h], self.b_q, tab=rope_h)
        for h in range(6):
            fm_chunk(C_KC + h * 128, 128, "rope", xchg[R_KC + h * 128:R_KC + (h + 1) * 128, :], self.b_xchg, tab=rope_h)
        if "fm_only" in DEBUG:
            return
        vb_dst = xchg[R_VB:R_VB + 128, :].rearrange("r (t d) -> (r t) d", d=128).rearrange("(i s) d -> s i d", s=128)
        vc_all = xchg[R_VC:R_VC + 768, :].rearrange("r c -> (r c)").rearrange("(t d) -> t d", d=768)
        pieces = [(C_VB, 128, "v", vb_dst)]
        for k in range(6):
            pieces.append((C_VC + k * 128, 128, "v", vc_all[:, k * 128:(k + 1) * 128].rearrange("(i s) d -> s i d", s=128)))
        pieces.append((C_WI, 16, "w", self.wi_d.rearrange("i s h -> s i h")))
        for (col, N, kind, dst) in pieces:
            s = cnt["w"] % NW
            cnt["w"] += 1
            P.dma(POOL, wch[s][:, :, 0:N], wv[:, :, col:col + N], reads=[b_win], writes=[wchb[s]])
            if kind == "v":
                k = cnt["t"] % 2
                cnt["t"] += 1
                for half in range(2):
                    bk = self.nb()
                    for ii in range(4):
                        i = half * 4 + ii
                        for kc in range(KC):
                            P.op(PE, lambda e, s=s, kc=kc, i=i, ii=ii, bk=bk: e.matmul(
                                banks[bk][:, ii * 128:(ii + 1) * 128], xn[:, kc, i * 128:(i + 1) * 128], wch[s][:, kc, 0:128],
                                start=(kc == 0), stop=(kc == KC - 1)),
                                reads=[wchb[s], xnb[kc]], writes=[bb[bk]])
                    P.op(ACT, lambda e, k=k, half=half, bk=bk: e.activation(
                        out=tst[k][:, half * 4:(half + 1) * 4, :].rearrange("p a b -> p (a b)"), in_=banks[bk], func=AF.Copy),
                        reads=[bb[bk]], writes=[tstb[k]])
                P.dma(SP, dst, tst[k], reads=[tstb[k]], writes=[self.b_xchg])
            else:
                bk = self.nb()
                for i in range(NSLOT):
                    for kc in range(KC):
                        P.op(PE, lambda e, s=s, kc=kc, i=i, bk=bk: e.matmul(
                            banks[bk][:, i * 16:(i + 1) * 16], xn[:, kc, i * 128:(i + 1) * 128], wch[s][:, kc, 0:16],
                            start=(kc == 0), stop=(kc == KC - 1)),
                            reads=[wchb[s], xnb[kc]], writes=[bb[bk]])
                P.op(ACT, lambda e, bk=bk: e.activation(out=wst.rearrange("p a b -> p (a b)"), in_=banks[bk][:, 0:128],
                                                       func=AF.Copy, scale=1.0 / 32.0),
                     reads=[bb[bk]], writes=[wstb])
                P.dma(SP, dst, wst, reads=[wstb], writes=[self.b_q])

    def attention(self, l):
        P, A = self.P, self.A
        banks, bb, cf, cb = self.banks, self.bb, self.cf, self.cb
        gath = self.gath
        A.reset(0)
        ki_e = A.take([SEQ], BF16)
        ki_o = A.take([SEQ], BF16)
        kbT = A.take([SEQ], BF16)
        vb = A.take([32, 128], BF16)
        maskT = A.take([32, 128], BF16)
        score = A.take([SEQ], F32)
        maskq = A.take([SEQ], BF16)
        scoreB = A.take([SEQ], F32)
        b_scoreB = Buf("scoreB")
        assert A.off <= KC * T * 4 + 16 * 1024
        A.reset(max(A.off, KC * T * 4))
        yT = A.take([KC, T], BF16)
        self.yT = yT
        self.yb = [Buf("y%d" % i) for i in range(NSLOT)]
        uh = A.take([4, 4, NSLOT, 2], BF16)
        b_kside = Buf("kside")
        b_uh = Buf("uh")
        NRL = 4
        rl = [A.take([512], F32) for _ in range(NRL)]
        rlb = [Buf("rl%d" % i) for i in range(NRL)]
        pe_ = [A.take([768], BF16) for _ in range(2)]
        peb = [Buf("pe0"), Buf("pe1")]
        pm = [A.take([768], BF16) for _ in range(2)]
        pmb = [Buf("pm0"), Buf("pm1")]
        qi_s = A.take([8, 128], BF16)
        qb_s = A.take([6, 128], BF16)
        qc_s = A.take([6, 128], BF16)
        wi_s = A.take([16], F32)
        uext = A.take([4, 130], BF16)
        gb_s = A.take([4, 128], BF16)
        cacc = A.take([128], F32)
        ctmp = A.take([128], F32)
        utmp = A.take([4, 2], BF16)
        stmp = A.take([512], F32)
        b_ctmp, b_utmp, b_stmp = Buf("ctmp"), Buf("utmp"), Buf("stmp")
        b_qi, b_qb, b_qc, b_wi, b_ue, b_gb, b_cacc = (Buf(n) for n in ("qi", "qb", "qc", "wi", "ue", "gb", "cacc"))
        NDK = 4
        kc_s = [A.take([2, 128], BF16) for _ in range(NDK)]
        vc_s = [A.take([256], BF16) for _ in range(NDK)]
        kcb = [Buf("kc%d" % i) for i in range(NDK)]
        vcb = [Buf("vc%d" % i) for i in range(NDK)]
        pe2 = [A.take([256], BF16) for _ in range(2)]
        pe2b = [Buf("pe2_0"), Buf("pe2_1")]
        pm2 = [A.take([256], BF16) for _ in range(2)]
        pm2b = [Buf("pm2_0"), Buf("pm2_1")]
        rden = A.take([768], F32)
        b_rden = Buf("rden")
        sm = A.take([8], F32)
        wk = A.take([NIT], F32)
        b_sm = Buf("sm")
        b_score, b_maskq, b_maskT = Buf("score"), Buf("maskq"), Buf("maskT")
        lo, hi, Rr, mid, cn, tt_ = (sm[:, k:k + 1] for k in range(6))
        self.att_end = A.off

        P.op(DVE, lambda e: e.memset(ki_e[64:128, :], 0.0), writes=[b_kside])
        P.op(DVE, lambda e: e.memset(ki_o[0:64, :], 0.0), writes=[b_kside])
        for rk in range(4):
            base = rk * XROWS

            def seqview(ap):
                return ap.rearrange("p (i r s) -> p i r s", r=4, s=128)[:, :, rk, :]
            src_ki = gath[base + R_KI:base + R_KI + 64, :].rearrange("d (i s) -> d i s", s=128)
            P.dma(SP, seqview(ki_e[0:64, :]), src_ki, reads=[self.b_gath], writes=[b_kside])
            P.dma(SP, seqview(ki_o[64:128, :]), src_ki, reads=[self.b_gath], writes=[b_kside])
            P.dma(SP, seqview(kbT), gath[base + R_KB:base + R_KB + 128, :].rearrange("d (i s) -> d i s", s=128),
                  reads=[self.b_gath], writes=[b_kside])
            vsrc = gath[base + R_VB:base + R_VB + 128, :].rearrange("r (t d) -> (r t) d", d=128).rearrange("(i s) d -> s i d", s=128)
            P.dma(SP, vb.rearrange("p (i r) d -> p i r d", r=4)[:, :, rk, :], vsrc, reads=[self.b_gath], writes=[b_kside])
            usrc = gath[base + R_UH:base + R_UH + 8, :].rearrange("r (a b) -> (r a) b", b=16).rearrange("(c p) (i t) -> p c i t", p=128, t=2)
            P.dma(SP, uh[:, rk], usrc, reads=[self.b_gath], writes=[b_uh])

        dmask = cb[:, CB_DIL:CB_DIL + NDIL * 128].rearrange("p (n q) -> p n q", q=128)
        dsam = cf[:, CF_DSA:CF_DSA + 512]
        li = l if self.mode == "F" else 0
        dcnt = 0
        for i in range(NSLOT):
            tsl = slice(i * 128, (i + 1) * 128)
            nblk = i + 1
            nkt = 4 * i + 4
            keys = nkt * 128
            P.dma(SP, qi_s, self.qi_d[:, :, tsl].rearrange("c p t -> p c t"), reads=[self.b_q], writes=[b_qi])
            P.dma(SP, qb_s, self.qb_d[:, :, tsl].rearrange("c p t -> p c t"), reads=[self.b_q], writes=[b_qb])
            P.dma(SP, qc_s, self.qc_d[:, :, tsl].rearrange("c p t -> p c t"), reads=[self.b_q], writes=[b_qc])
            P.dma(SP, wi_s, self.wi_d[i], reads=[self.b_q], writes=[b_wi])
            P.dma(SP, uext[:, :, 2:130], self.u_d[:, :, tsl].rearrange("c p t -> p c t"), reads=[self.b_q], writes=[b_ue])
            P.dma(SP, gb_s, self.gb_d[:, :, tsl].rearrange("c p t -> p c t"), reads=[self.b_q], writes=[b_gb])
            first = True
            for j in range(4):
                if j == 0:
                    if i == 0:
                        continue
                    cand = uh[:, 3, :, i - 1, :]
                else:
                    cand = uh[:, j - 1, :, i, :]
                selj = cf[:, CF_SEL + j:CF_SEL + j + 1]
                if first:
                    P.op(POOL, lambda e, cand=cand, selj=selj: e.tensor_scalar(out=uext[:, :, 0:2], in0=cand, scalar1=selj,
                                                                              scalar2=None, op0=ALU.mult),
                         reads=[b_uh, self.b_cst], writes=[b_ue])
                    first = False
                else:
                    P.op(POOL, lambda e, cand=cand, selj=selj: e.tensor_scalar(out=utmp, in0=cand, scalar1=selj,
                                                                              scalar2=None, op0=ALU.mult),
                         reads=[b_uh, self.b_cst], writes=[b_utmp])
                    P.op(POOL, lambda e: e.tensor_tensor(out=uext[:, :, 0:2], in0=uext[:, :, 0:2], in1=utmp, op=ALU.add),
                         reads=[b_utmp, b_ue], writes=[b_ue])
            for c in range(4):
                def cw(j, c=c):
                    col = CF_CONV + (li * 3 + j) * 4 + c
                    return cf[:, col:col + 1]
                P.op(POOL, lambda e, c=c, cw=cw: e.tensor_scalar(out=cacc, in0=uext[:, c, 0:128], scalar1=cw(0), scalar2=None,
                                                                op0=ALU.mult), reads=[b_ue, self.b_cst], writes=[b_cacc])
                for j in (1, 2):
                    P.op(POOL, lambda e, c=c, j=j, cw=cw: e.tensor_scalar(out=ctmp, in0=uext[:, c, j:j + 128], scalar1=cw(j),
                                                                         scalar2=None, op0=ALU.mult),
                         reads=[b_ue, self.b_cst], writes=[b_ctmp])
                    P.op(POOL, lambda e: e.tensor_tensor(out=cacc, in0=cacc, in1=ctmp, op=ALU.add),
                         reads=[b_ctmp, b_cacc], writes=[b_cacc])
                P.op(POOL, lambda e, c=c, tsl=tsl: e.tensor_tensor(out=yT[:, c, tsl], in0=cacc, in1=gb_s[:, c, :], op=ALU.mult),
                     reads=[b_cacc, b_gb], writes=[self.yb[i]])
            for b in range(nblk):
                ksl = slice(b * 512, (b + 1) * 512)
                for h in range(16):
                    k = (b * 16 + h) % NRL
                    kside = ki_e if h % 2 == 0 else ki_o
                    P.op(PE, lambda e, h=h, ksl=ksl, kside=kside: e.matmul(banks[6], qi_s[:, h // 2, :], kside[:, ksl],
                                                                          start=True, stop=True),
                         reads=[b_qi, b_kside], writes=[bb[6]])
                    P.op(ACT, lambda e, k=k: e.activation(out=rl[k], in_=banks[6], func=AF.Relu),
                         reads=[bb[6]], writes=[rlb[k]])
                    if h == 0:
                        P.op(DVE, lambda e, k=k, ksl=ksl: e.tensor_scalar(out=score[:, ksl], in0=rl[k], scalar1=wi_s[:, 0:1],
                                                                         scalar2=None, op0=ALU.mult),
                             reads=[rlb[k], b_wi], writes=[b_score])
                    elif h == 1:
                        P.op(POOL, lambda e, k=k, ksl=ksl: e.tensor_scalar(out=scoreB[:, ksl], in0=rl[k], scalar1=wi_s[:, 1:2],
                                                                          scalar2=None, op0=ALU.mult),
                             reads=[rlb[k], b_wi], writes=[b_scoreB])
                    elif h % 2 == 0:
                        P.op(DVE, lambda e, k=k, ksl=ksl, h=h: e.scalar_tensor_tensor(
                            out=score[:, ksl], in0=rl[k], scalar=wi_s[:, h:h + 1], in1=score[:, ksl], op0=ALU.mult, op1=ALU.add),
                            reads=[rlb[k], b_wi, b_score], writes=[b_score])
                    else:
                        P.op(POOL, lambda e, k=k, h=h: e.tensor_scalar(out=stmp, in0=rl[k], scalar1=wi_s[:, h:h + 1],
                                                                      scalar2=None, op0=ALU.mult),
                             reads=[rlb[k], b_wi], writes=[b_stmp])
                        P.op(POOL, lambda e, ksl=ksl: e.tensor_tensor(out=scoreB[:, ksl], in0=scoreB[:, ksl], in1=stmp, op=ALU.add),
                             reads=[b_stmp, b_scoreB], writes=[b_scoreB])
                P.op(DVE, lambda e, ksl=ksl: e.tensor_tensor(out=score[:, ksl], in0=score[:, ksl], in1=scoreB[:, ksl], op=ALU.add),
                     reads=[b_score, b_scoreB], writes=[b_score])
            sc = score[:, 0:keys]
            P.op(DVE, lambda e, sc=sc: e.tensor_reduce(out=lo, in_=sc, axis=AX.X, op=ALU.min), reads=[b_score], writes=[b_sm])
            P.op(DVE, lambda e: e.tensor_scalar(out=lo, in0=lo, scalar1=-1.0, scalar2=None, op0=ALU.add), reads=[b_sm], writes=[b_sm])
            lsl = slice(keys - 512, keys)
            P.op(DVE, lambda e, lsl=lsl: e.tensor_tensor(out=score[:, lsl], in0=score[:, lsl], in1=dsam, op=ALU.add),
                 reads=[b_score, self.b_cst, b_sm], writes=[b_score])
            P.op(DVE, lambda e, sc=sc: e.tensor_reduce(out=hi, in_=sc, axis=AX.X, op=ALU.max), reads=[b_score], writes=[b_sm])
            P.op(DVE, lambda e: e.tensor_tensor(out=Rr, in0=hi, in1=lo, op=ALU.subtract), reads=[b_sm], writes=[b_sm])
            P.op(DVE, lambda e: e.tensor_scalar(out=wk, in0=cf[:, CF_PW2:CF_PW2 + NIT], scalar1=Rr, scalar2=None, op0=ALU.mult),
                 reads=[b_sm, self.b_cst], writes=[b_sm])
            mq = maskq[:, 0:keys]
            for k in range(NIT):
                P.op(DVE, lambda e, k=k: e.tensor_tensor(out=mid, in0=lo, in1=wk[:, k:k + 1], op=ALU.add), reads=[b_sm], writes=[b_sm])
                P.op(DVE, lambda e, sc=sc, mq=mq: e.tensor_scalar(out=mq, in0=sc, scalar1=mid, scalar2=0.0, op0=ALU.is_gt,
                                                                 op1=ALU.add, accum_out=cn),
                     reads=[b_score, b_sm], writes=[b_maskq, b_sm])
                P.op(DVE, lambda e, k=k: e.tensor_scalar(out=tt_, in0=cn, scalar1=TOPK - 0.5, scalar2=wk[:, k:k + 1],
                                                        op0=ALU.is_gt, op1=ALU.mult), reads=[b_sm], writes=[b_sm])
                P.op(DVE, lambda e: e.tensor_tensor(out=lo, in0=lo, in1=tt_, op=ALU.add), reads=[b_sm], writes=[b_sm])
            P.op(DVE, lambda e, sc=sc, mq=mq: e.tensor_scalar(out=mq, in0=sc, scalar1=lo, scalar2=None, op0=ALU.is_gt),
                 reads=[b_score, b_sm], writes=[b_maskq])
            pT = banks[7].bitcast(BF16).rearrange("p (a b) -> p a b", b=128)
            for b in range(nblk):
                for j in range(4):
                    kt = b * 4 + j
                    P.op(PE, lambda e, kt=kt, j=j: e.transpose(pT[:, j, :], maskq[:, kt * 128:(kt + 1) * 128], self.ident),
                         reads=[b_maskq, self.b_cst], writes=[bb[7]])
                P.op(ACT, lambda e, b=b: e.activation(out=maskT[:, b * 4:(b + 1) * 4, :].rearrange("p a b -> p (a b)"),
                                                     in_=pT[:, 0:4, :].rearrange("p a b -> p (a b)"), func=AF.Copy),
                     reads=[bb[7]], writes=[b_maskT])
            qb_f = qb_s.rearrange("p a b -> p (a b)")
            for kt in range(nkt):
                k = kt % 2
                for half in range(2):
                    P.op(PE, lambda e, kt=kt, half=half: e.matmul(banks[half][:, 0:384], kbT[:, kt * 128:(kt + 1) * 128],
                                                                 qb_f[:, half * 384:(half + 1) * 384], start=True, stop=True),
                         reads=[b_kside, b_qb], writes=[bb[half]])
                for half in range(2):
                    P.op(ACT, lambda e, k=k, half=half: e.activation(out=pe_[k][:, half * 384:(half + 1) * 384],
                                                                    in_=banks[half][:, 0:384], func=AF.Exp, scale=ATT_SCALE),
                         reads=[bb[half]], writes=[peb[k]])
                P.op(DVE, lambda e, k=k, kt=kt: e.tensor_tensor(
                    out=pm[k].rearrange("p (a b) -> p a b", b=128), in0=pe_[k].rearrange("p (a b) -> p a b", b=128),
                    in1=maskT[:, kt, :].unsqueeze(1).to_broadcast([128, 6, 128]), op=ALU.mult),
                    reads=[peb[k], b_maskT], writes=[pmb[k]])
                for half in range(2):
                    P.op(PE, lambda e, k=k, kt=kt, half=half: e.matmul(banks[2 + half][:, 0:384], vb[:, kt, :],
                                                                      pm[k][:, half * 384:(half + 1) * 384],
                                                                      start=(kt == 0), stop=(kt == nkt - 1)),
                         reads=[b_kside, pmb[k]], writes=[bb[2 + half]])
                    P.op(PE, lambda e, k=k, kt=kt, half=half: e.matmul(banks[4 + half][:, 0:384], self.ones,
                                                                      pm[k][:, half * 384:(half + 1) * 384],
                                                                      start=(kt == 0), stop=(kt == nkt - 1)),
                         reads=[self.b_cst, pmb[k]], writes=[bb[4 + half]])
            for half in range(2):
                P.op(DVE, lambda e, half=half: e.reciprocal(out=rden[:, half * 384:(half + 1) * 384], in_=banks[4 + half][:, 0:384]),
                     reads=[bb[4 + half]], writes=[b_rden])
                P.op(DVE, lambda e, half=half, tsl=tsl: e.tensor_tensor(
                    out=yT[:, 4 + 3 * half:7 + 3 * half, tsl], in0=banks[2 + half][:, 0:384].rearrange("p (a b) -> p a b", b=128),
                    in1=rden[:, half * 384:(half + 1) * 384].rearrange("p (a b) -> p a b", b=128), op=ALU.mult),
                    reads=[bb[2 + half], b_rden], writes=[self.yb[i]])
            steps = []
            for di, (g, kp) in enumerate(DIL_LIST):
                Tk = 4 * i - DIL_X[g] + kp
                if Tk >= 0:
                    steps.append((di, g, Tk))
            started = {2: False, 3: False}
            nsteps = len(steps)
            last_of_region = {}
            for n, (di, g, Tk) in enumerate(steps):
                for hh in range(2):
                    last_of_region[2 * g + hh] = n
            for n, (di, g, Tk) in enumerate(steps):
                rk, ik = Tk % 4, Tk // 4
                base = rk * XROWS
                s = dcnt % NDK
                k = dcnt % 2
                dcnt += 1
                ksrc = gath[base + R_KC + 2 * g * 128:base + R_KC + (2 * g + 2) * 128, ik * 128:(ik + 1) * 128].rearrange(
                    "(h d) s -> d h s", d=128)
                P.dma(SP, kc_s[s], ksrc, reads=[self.b_gath], writes=[kcb[s]])
                vsrc = gath[base + R_VC:base + R_VC + 768, :].rearrange("r c -> (r c)").rearrange("(t d) -> t d", d=768)[
                    ik * 128:(ik + 1) * 128, 2 * g * 128:(2 * g + 2) * 128]
                P.dma(SP, vc_s[s], vsrc, reads=[self.b_gath], writes=[vcb[s]])
                for hh in range(2):
                    P.op(PE, lambda e, s=s, hh=hh, g=g: e.matmul(banks[0][:, hh * 128:(hh + 1) * 128], kc_s[s][:, hh, :],
                                                                qc_s[:, 2 * g + hh, :], start=True, stop=True),
                         reads=[kcb[s], b_qc], writes=[bb[0]])
                P.op(ACT, lambda e, k=k: e.activation(out=pe2[k], in_=banks[0][:, 0:256], func=AF.Exp, scale=ATT_SCALE),
                     reads=[bb[0]], writes=[pe2b[k]])
                P.op(DVE, lambda e, k=k, di=di: e.tensor_tensor(
                    out=pm2[k].rearrange("p (a b) -> p a b", b=128), in0=pe2[k].rearrange("p (a b) -> p a b", b=128),
                    in1=dmask[:, di, :].unsqueeze(1).to_broadcast([128, 2, 128]), op=ALU.mult),
                    reads=[pe2b[k], self.b_cst], writes=[pm2b[k]])
                for hh in range(2):
                    idx6 = 2 * g + hh
                    bk = 2 + idx6 // 3
                    col = (idx6 % 3) * 128
                    st_ = not started[bk]
                    started[bk] = True
                    P.op(PE, lambda e, s=s, k=k, hh=hh, bk=bk, col=col, st_=st_, idx6=idx6, n=n: e.matmul(
                        banks[bk][:, col:col + 128], vc_s[s][:, hh * 128:(hh + 1) * 128], pm2[k][:, hh * 128:(hh + 1) * 128],
                        start=st_, stop=(last_of_region[idx6] == n), skip_group_check=True),
                        reads=[vcb[s], pm2b[k]], writes=[bb[bk]])
                P.op(PE, lambda e, k=k, n=n: e.matmul(banks[4][:, 0:256], self.ones, pm2[k], start=(n == 0), stop=(n == nsteps - 1)),
                     reads=[self.b_cst, pm2b[k]], writes=[bb[4]])
            P.op(DVE, lambda e: e.reciprocal(out=rden[:, 0:256], in_=banks[4][:, 0:256]), reads=[bb[4]], writes=[b_rden])
            for idx6 in range(6):
                bk = 2 + idx6 // 3
                col = (idx6 % 3) * 128
                hh = idx6 % 2
                P.op(DVE, lambda e, idx6=idx6, bk=bk, col=col, hh=hh, tsl=tsl: e.tensor_tensor(
                    out=yT[:, 10 + idx6, tsl], in0=banks[bk][:, col:col + 128], in1=rden[:, hh * 128:(hh + 1) * 128], op=ALU.mult),
                    reads=[bb[bk], b_rden], writes=[self.yb[i]])

    def outproj(self, l):
        P, A = self.P, self.A
        banks, bb = self.banks, self.bb
        yT = self.yT
        xT = self.xT
        NW = 3
        A.reset(self.att_end)
        wo = [A.take([KC, 128], BF16) for _ in range(NW)]
        wob = [Buf("wo%d" % i) for i in range(NW)]
        wl = self.w_outp[l if self.mode == "F" else 0].rearrange("(kc p) n -> p kc n", p=128)
        for dc in range(KC):
            s = dc % NW
            P.dma(POOL, wo[s], wl[:, :, dc * 128:(dc + 1) * 128], reads=[self.wbuf[("w_out", l if self.mode == "F" else 0)]], writes=[wob[s]])
            bk = [self.nb(), self.nb()]
            for kc in range(KC):
                for tt in range(2):
                    P.op(PE, lambda e, s=s, kc=kc, tt=tt, bk=bk: e.matmul(
                        banks[bk[tt]], wo[s][:, kc, :], yT[:, kc, tt * 512:(tt + 1) * 512], start=(kc == 0), stop=(kc == KC - 1)),
                        reads=[wob[s]] + self.yb[tt * 4:(tt + 1) * 4], writes=[bb[bk[tt]]])
            for tt in range(2):
                P.op(DVE, lambda e, dc=dc, tt=tt, bk=bk: e.tensor_tensor(
                    out=xT[:, dc, tt * 512:(tt + 1) * 512], in0=banks[bk[tt]], in1=xT[:, dc, tt * 512:(tt + 1) * 512], op=ALU.add),
                    reads=[bb[bk[tt]], self.xb[dc]], writes=[self.xb[dc]])

    def final_norm_store(self):
        P, A = self.P, self.A
        xT = self.xT
        A.reset(KC * T * 4)
        ob = [A.take([T], F32) for _ in range(2)]
        obb = [Buf("ob0"), Buf("ob1")]

        def norm_out(c, rstd, rb, g):
            s = c % 2
            P.op(DVE, lambda e: e.scalar_tensor_tensor(out=ob[s], in0=xT[:, c, :], scalar=g[:, c:c + 1], in1=rstd,
                                                      op0=ALU.mult, op1=ALU.mult),
                 reads=[self.xb[c], rb, self.b_cst], writes=[obb[s]])
            P.dma(SP, self.x_out[c], ob[s], reads=[obb[s]])
        self.rmsnorm(self.gain(12), norm_out)

    def build(self, last=False):
        P = self.P
        mode = self.mode
        if mode == "A":
            self.carve_x()
            self.load_x(self.x_in)
            if "noffn" not in DEBUG:
                self.ffn(self.w_ffn1, 0, 0, self.n_ffn1)
            P.barrier()
            if "noinproj" not in DEBUG:
                self.inproj(0)
            self.store_x(self.x_d, self.b_xd)
        elif mode == "B":
            self.attention(0)
            P.barrier()
            self.carve_x()
            self.load_x(self.x_d)
            self.outproj(0)
            P.barrier()
            if "noffn2" not in DEBUG:
                self.ffn(self.w_ffn2, 0, 2, self.n_ffn2)
                P.barrier()
            if last:
                self.final_norm_store()
            else:
                self.store_x(self.x_out)
        else:
            self.carve_x()
            self.load_x(self.x_in)
            for l in range(DEPTH):
                self.ffn(self.w_ffn1, l, l * 3 + 0, self.n_ffn1)
                P.barrier()
                self.inproj(l)
                self.store_x(self.x_d, self.b_xd)
                P.op(POOL, lambda e: e.collective_compute("AllGather", ALU.bypass, replica_groups=[[0, 1, 2, 3], [4, 5, 6, 7]],
                                                         ins=[self.xchg.opt()], outs=[self.gath.opt()]),
                     reads=[self.b_xchg], writes=[self.b_gath], cc=True)
                P.barrier()
                self.attention(l)
                P.barrier()
                self.carve_x()
                self.load_x(self.x_d)
                self.outproj(l)
                P.barrier()
                self.ffn(self.w_ffn2, l, l * 3 + 2, self.n_ffn2)
                P.barrier()
            self.final_norm_store()
        P.barrier()
        P.emit()
        P.close()
        return self.nc


def _consts(core, norms, conv_w):
    r = core % 4
    cbm = np.zeros((128, NCB), dtype=np.float32)
    cbm[:, CB_ONES:CB_ONES + 128] = 1.0
    cbm[:, CB_ID:CB_ID + 128] = np.eye(128, dtype=np.float32)
    Rh = np.zeros((128, 128), dtype=np.float32)
    for m in range(64):
        Rh[m + 64, m] = -1.0
        Rh[m, m + 64] = 1.0
    Ri = np.zeros((128, 128), dtype=np.float32)
    for b0 in (0, 64):
        for m in range(32):
            Ri[b0 + m + 32, b0 + m] = -1.0
            Ri[b0 + m, b0 + m + 32] = 1.0
    cbm[:, CB_RH:CB_RH + 128] = Rh
    cbm[:, CB_RI:CB_RI + 128] = Ri
    s_idx = np.arange(128)[:, None]
    q_idx = np.arange(128)[None, :]
    for di, (g, kp) in enumerate(DIL_LIST):
        delta = r + DIL_X[g] - kp
        dist = 128 * delta + (q_idx - s_idx)
        ok = (dist >= 0) & (dist <= DIL_WIN[g]) & (dist % DIL_DIL[g] == 0)
        cbm[:, CB_DIL + di * 128:CB_DIL + (di + 1) * 128] = ok.astype(np.float32)
    cfm = np.zeros((128, NCF), dtype=np.float32)
    for n in range(13):
        cfm[:, CF_GAIN + n * 16:CF_GAIN + (n + 1) * 16] = norms[n].reshape(KC, 128).T
    for l in range(DEPTH):
        for j in range(3):
            cfm[:, CF_CONV + (l * 3 + j) * 4:CF_CONV + (l * 3 + j) * 4 + 4] = conv_w[l, j].reshape(4, 128).T
    inv_h = (1.0 / (np.float32(10000.0) ** (np.arange(0, 128, 2, dtype=np.float32) / np.float32(128)))).astype(np.float32)
    inv_i = (1.0 / (np.float32(10000.0) ** (np.arange(0, 64, 2, dtype=np.float32) / np.float32(64)))).astype(np.float32)
    p = np.arange(128)
    cfm[:, CF_INV] = inv_h[p % 64]
    cfm[:, CF_INV + 1] = inv_i[p % 32]
    cfm[:, CF_SEL + r] = 1.0
    cfm[:, CF_PW2:CF_PW2 + NIT] = (0.5 ** (np.arange(NIT) + 1)).astype(np.float32)[None, :]
    dm = np.zeros((128, 512), dtype=np.float32)
    for j in range(4):
        blk = dm[:, j * 128:(j + 1) * 128]
        if j > r:
            blk[:] = NEG
        elif j == r:
            blk[:] = np.where(np.arange(128)[None, :] <= np.arange(128)[:, None], 0.0, NEG)
    cfm[:, CF_DSA:CF_DSA + 512] = dm
    return cbm.astype(NPBF), cfm


def _core_tokens(core):
    r = core % 4
    idx = np.concatenate([np.arange((4 * i + r) * 128, (4 * i + r + 1) * 128) for i in range(NSLOT)])
    return core // 4, idx


_PROGS = {}


def _get_prog(mode, last=False):
    key = (mode, last)
    if key not in _PROGS:
        _PROGS[key] = Builder(mode).build(last=last)
    return _PROGS[key]


FUSED = False


def kernel(x, positions, norm_ffn1, ffn1_gate, ffn1_up, ffn1_down, norm_mix, w_in, conv_w, w_out,
           norm_ffn2, ffn2_gate, ffn2_up, ffn2_down, norm_final):
    x = np.asarray(x, dtype=np.float32)
    positions = np.asarray(positions)
    norms = []
    for l in range(DEPTH):
        norms += [np.asarray(norm_ffn1[l]), np.asarray(norm_mix[l]), np.asarray(norm_ffn2[l])]
    norms.append(np.asarray(norm_final))
    conv_w = np.asarray(conv_w, dtype=np.float32)
    cores = list(range(NCORE))
    xT, pos, cbs, cfs = [], [], [], []
    for c in cores:
        b, idx = _core_tokens(c)
        xT.append(np.ascontiguousarray(x[b, idx, :].T.reshape(KC, 128, T)))
        pos.append(np.ascontiguousarray(positions[b, idx].astype(np.int32).reshape(1, T)))
        cbm, cfm = _consts(c, norms, conv_w)
        cbs.append(cbm)
        cfs.append(cfm)
    def wsh(w, c, l=None):
        w = np.asarray(w)
        if l is not None:
            w = w[l:l + 1]
        if not WSHARD:
            return w
        n = w.shape[1] // NCORE
        return np.ascontiguousarray(w[:, c * n:(c + 1) * n])
    if FUSED:
        nc = _get_prog("F")
        in_maps = []
        for c in cores:
            in_maps.append({"cst_bf": cbs[c], "cst_f32": cfs[c], "x_in": xT[c], "pos": pos[c],
                            "ffn1_gate": wsh(ffn1_gate, c), "ffn1_up": wsh(ffn1_up, c), "ffn1_down": wsh(ffn1_down, c),
                            "w_in": wsh(w_in, c), "ffn2_gate": wsh(ffn2_gate, c), "ffn2_up": wsh(ffn2_up, c),
                            "ffn2_down": wsh(ffn2_down, c), "w_out": wsh(w_out, c)})
        res = run_bass_kernel_spmd(nc, in_maps, core_ids=cores)
        outs = [res.results[c]["x_out"] for c in cores]
    else:
        cur = xT
        outs = None
        for l in range(DEPTH):
            cf_l = []
            for c in cores:
                m = cfs[c].copy()
                for n in range(3):
                    m[:, CF_GAIN + n * 16:CF_GAIN + (n + 1) * 16] = cfs[c][:, CF_GAIN + (l * 3 + n) * 16:CF_GAIN + (l * 3 + n + 1) * 16]
                m[:, CF_CONV:CF_CONV + 12] = cfs[c][:, CF_CONV + l * 12:CF_CONV + (l + 1) * 12]
                cf_l.append(m)
            ncA = _get_prog("A")
            in_maps = [{"cst_bf": cbs[c], "cst_f32": cf_l[c], "x_in": cur[c], "pos": pos[c],
                        "ffn1_gate": wsh(ffn1_gate, c, l), "ffn1_up": wsh(ffn1_up, c, l), "ffn1_down": wsh(ffn1_down, c, l),
                        "w_in": wsh(w_in, c, l)} for c in cores]
            ra = run_bass_kernel_spmd(ncA, in_maps, core_ids=cores).results
            last = l == DEPTH - 1
            ncB = _get_prog("B", last)
            in_maps = []
            for c in cores:
                g0 = (c // 4) * 4
                gath = np.concatenate([ra[g0 + k]["xchg"] for k in range(4)], axis=0)
                in_maps.append({"cst_bf": cbs[c], "cst_f32": cf_l[c], "gath": gath,
                                "x_d": ra[c]["x_d"], "qb_d": ra[c]["qb_d"], "qi_d": ra[c]["qi_d"], "qc_d": ra[c]["qc_d"],
                                "wi_d": ra[c]["wi_d"], "u_d": ra[c]["u_d"], "gb_d": ra[c]["gb_d"],
                                "ffn2_gate": wsh(ffn2_gate, c, l), "ffn2_up": wsh(ffn2_up, c, l), "ffn2_down": wsh(ffn2_down, c, l),
                                "w_out": wsh(w_out, c, l)})
            rb = run_bass_kernel_spmd(ncB, in_maps, core_ids=cores).results
            cur = [rb[c]["x_out"] for c in cores]
        outs = cur
    out = np.empty((2, SEQ, D), dtype=np.float32)
    for c in cores:
        b, idx = _core_tokens(c)
        out[b, idx, :] = outs[c].reshape(D, T).T
    return out
```
